# Optimizing a Trainium2 kernel written in Bass

```python
import math
import jax, jax.numpy as jnp
from jax import lax
import numpy as np

D_MODEL = 2048
BATCH = 2
SEQ = 16384
DEPTH = 1

ATTN_HEADS = 8
ATTN_HEAD_DIM = 128
ATTN_WIDTH = ATTN_HEADS * ATTN_HEAD_DIM
Q_BLOCK = 128
SSM_WIDTH = D_MODEL // 4
SSM_GROUP = 16
SSM_GROUPS = SSM_WIDTH // SSM_GROUP
SSM_STATE = 64
DT_MIN = 1e-3
DT_MAX = 1e-1
N_BRANCHES = 2
PEER_HEADS = 8
PEER_N_KEYS = 128
PEER_N_EXPERTS = PEER_N_KEYS * PEER_N_KEYS
PEER_QUERY_DIM = 256
PEER_HALF_DIM = PEER_QUERY_DIM // 2
PEER_TOPK = 16
PEER_CHUNK = 128
N_MOD = 6
RMS_EPS = 1e-6
NEG_INF = -1e30

IN_SIZES = (ATTN_WIDTH, ATTN_WIDTH, ATTN_WIDTH, ATTN_HEADS, SSM_WIDTH, D_MODEL, D_MODEL)
IN_WIDTH = sum(IN_SIZES)
IN_SPLIT_POINTS = tuple(int(v) for v in np.cumsum(IN_SIZES)[:-1])

kernel_name = "hybrid_s5_fox_peer_block"


def rms_norm(x, w):
    xf = x.astype(jnp.float32)
    xf = xf * lax.rsqrt(jnp.mean(xf * xf, axis=-1, keepdims=True) + RMS_EPS)
    return xf.astype(x.dtype) * w


def modulate(h, shift, scale):
    return h * (1.0 + scale[:, None, :]) + shift[:, None, :]


def _ssm_combine(e_i, e_j):
    ar_i, ai_i, br_i, bi_i = e_i
    ar_j, ai_j, br_j, bi_j = e_j
    ar = ar_j * ar_i - ai_j * ai_i
    ai = ar_j * ai_i + ai_j * ar_i
    br = ar_j * br_i - ai_j * bi_i + br_j
    bi = ar_j * bi_i + ai_j * br_i + bi_j
    return (ar, ai, br, bi)


def s5_mixer(u, A_re, A_im, log_dt, B_re, B_im, C_re, C_im, D_skip):
    bsz, seq, _ = u.shape
    f32 = jnp.float32
    uf = u.astype(f32).reshape(bsz, seq, SSM_GROUPS, SSM_GROUP)
    lam_re = A_re.astype(f32)
    lam_im = A_im.astype(f32)
    dt = jnp.exp(log_dt.astype(f32))[:, None]
    mag = jnp.exp(lam_re * dt)
    ab_re = mag * jnp.cos(lam_im * dt)
    ab_im = mag * jnp.sin(lam_im * dt)
    den = lam_re * lam_re + lam_im * lam_im
    nr = ab_re - 1.0
    coef_re = (nr * lam_re + ab_im * lam_im) / den
    coef_im = (ab_im * lam_re - nr * lam_im) / den
    b_re = B_re.astype(f32)
    b_im = B_im.astype(f32)
    bb_re = coef_re[..., None] * b_re - coef_im[..., None] * b_im
    bb_im = coef_re[..., None] * b_im + coef_im[..., None] * b_re
    bu_re = jnp.einsum('bsgc,gpc->bsgp', uf, bb_re)
    bu_im = jnp.einsum('bsgc,gpc->bsgp', uf, bb_im)
    a_re = jnp.broadcast_to(ab_re, (1, seq, SSM_GROUPS, SSM_STATE))
    a_im = jnp.broadcast_to(ab_im, (1, seq, SSM_GROUPS, SSM_STATE))
    _, _, st_re, st_im = lax.associative_scan(_ssm_combine, (a_re, a_im, bu_re, bu_im), axis=1)
    y = (jnp.einsum('gcp,bsgp->bsgc', C_re.astype(f32), st_re)
         - jnp.einsum('gcp,bsgp->bsgc', C_im.astype(f32), st_im)
         + D_skip.astype(f32) * uf)
    return y.reshape(bsz, seq, SSM_WIDTH).astype(u.dtype)


def half_glu(y, w_glu, b_glu):
    g = jax.nn.gelu(y, approximate=False)
    return g * jax.nn.sigmoid(g @ w_glu + b_glu)


def head_rms(t, w):
    tf = t.astype(jnp.float32)
    tf = tf * lax.rsqrt(jnp.mean(tf * tf, axis=-1, keepdims=True) + RMS_EPS)
    return tf.astype(t.dtype) * w


def fox_attention(q, k, v, f_logit, b_forget, q_norm_w, k_norm_w):
    bsz, seq, _ = q.shape
    q = head_rms(q.reshape(bsz, seq, ATTN_HEADS, ATTN_HEAD_DIM), q_norm_w) * (ATTN_HEAD_DIM ** -0.5)
    k = head_rms(k.reshape(bsz, seq, ATTN_HEADS, ATTN_HEAD_DIM), k_norm_w)
    v = v.reshape(bsz, seq, ATTN_HEADS, ATTN_HEAD_DIM)
    log_f = jax.nn.log_sigmoid((f_logit + b_forget).astype(jnp.float32))
    cum = jnp.cumsum(log_f, axis=1).transpose(0, 2, 1)
    nb = seq // Q_BLOCK
    q_blocks = q.reshape(bsz, nb, Q_BLOCK, ATTN_HEADS, ATTN_HEAD_DIM).transpose(1, 0, 2, 3, 4)
    cum_blocks = cum.reshape(bsz, ATTN_HEADS, nb, Q_BLOCK).transpose(2, 0, 1, 3)
    kpos = jnp.arange(seq)

    def block(args):
        qb, cq, bi = args
        s = (jnp.einsum('bqhd,bkhd->bhqk', qb, k).astype(jnp.float32)
             + cq[..., None] - cum[:, :, None, :])
        qpos = bi * Q_BLOCK + jnp.arange(Q_BLOCK)
        s = jnp.where(kpos[None, :] <= qpos[:, None], s, NEG_INF)
        p = jax.nn.softmax(s, axis=-1).astype(v.dtype)
        return jnp.einsum('bhqk,bkhd->bqhd', p, v)

    o = lax.map(block, (q_blocks, cum_blocks, jnp.arange(nb)))
    return o.transpose(1, 0, 2, 3, 4).reshape(bsz, seq, ATTN_WIDTH)


def peer_ffn(h, w_q, sub_keys, expert_u, expert_v):
    bsz, seq, d = h.shape
    q = (h @ w_q).reshape(bsz, seq, PEER_HEADS, 2, PEER_HALF_DIM)
    scores = jnp.einsum('bshpd,hpnd->bshpn', q, sub_keys).astype(jnp.float32)
    sv, si = lax.top_k(scores, PEER_TOPK)
    cand = (sv[..., 0, :, None] + sv[..., 1, None, :]).reshape(bsz, seq, PEER_HEADS, PEER_TOPK * PEER_TOPK)
    cidx = (si[..., 0, :, None] * PEER_N_KEYS + si[..., 1, None, :]).reshape(bsz, seq, PEER_HEADS, PEER_TOPK * PEER_TOPK)
    tv, tp = lax.top_k(cand, PEER_TOPK)
    eidx = jnp.take_along_axis(cidx, tp, axis=-1)
    gw = jax.nn.softmax(tv, axis=-1).astype(h.dtype)
    n_sel = PEER_HEADS * PEER_TOPK
    nc = (bsz * seq) // PEER_CHUNK
    xs = h.reshape(nc, PEER_CHUNK, d)
    ids = eidx.reshape(nc, PEER_CHUNK, n_sel)
    ws = gw.reshape(nc, PEER_CHUNK, n_sel)

    def chunk(args):
        xc, ic, wc = args
        a = jnp.einsum('td,ted->te', xc, expert_u[ic])
        a = jax.nn.gelu(a, approximate=False) * wc
        return jnp.einsum('te,ted->td', a, expert_v[ic])

    out = lax.map(chunk, (xs, ids, ws))
    return out.reshape(bsz, seq, d)


def setup_inputs(seed: int = 0) -> dict:
    key = jax.random.key(seed)
    ks = jax.random.split(key, 32)
    f32 = jnp.float32
    L = DEPTH
    nrm = lambda k, shape, s: (jax.random.normal(k, shape, f32) * s)
    n_idx = jnp.arange(SSM_STATE, dtype=f32)
    A_re = -0.5 * jnp.exp(nrm(ks[9], (L, SSM_GROUPS, SSM_STATE), 0.05))
    A_im = math.pi * n_idx[None, None, :] + nrm(ks[10], (L, SSM_GROUPS, SSM_STATE), 0.01)
    log_dt = jax.random.uniform(ks[11], (L, SSM_GROUPS), f32, math.log(DT_MIN), math.log(DT_MAX))
    return {
        "x": nrm(ks[0], (BATCH, SEQ, D_MODEL), 1.0),
        "c": nrm(ks[1], (BATCH, D_MODEL), 1.0),
        "w_ada": nrm(ks[2], (L, D_MODEL, N_MOD * D_MODEL), 0.02),
        "b_ada": nrm(ks[3], (L, N_MOD * D_MODEL), 0.01),
        "norm1_w": 1.0 + nrm(ks[4], (L, D_MODEL), 0.02),
        "w_in": nrm(ks[5], (L, D_MODEL, IN_WIDTH), D_MODEL ** -0.5),
        "b_forget": jax.random.uniform(ks[6], (L, ATTN_HEADS), f32, 1.0, 3.0),
        "q_norm_w": 1.0 + nrm(ks[7], (L, ATTN_HEAD_DIM), 0.02),
        "k_norm_w": 1.0 + nrm(ks[8], (L, ATTN_HEAD_DIM), 0.02),
        "ssm_A_re": A_re,
        "ssm_A_im": A_im,
        "ssm_log_dt": log_dt,
        "ssm_B_re": nrm(ks[12], (L, SSM_GROUPS, SSM_STATE, SSM_GROUP), (2.0 * SSM_GROUP) ** -0.5),
        "ssm_B_im": nrm(ks[13], (L, SSM_GROUPS, SSM_STATE, SSM_GROUP), (2.0 * SSM_GROUP) ** -0.5),
        "ssm_C_re": nrm(ks[14], (L, SSM_GROUPS, SSM_GROUP, SSM_STATE), (2.0 * SSM_STATE) ** -0.5),
        "ssm_C_im": nrm(ks[15], (L, SSM_GROUPS, SSM_GROUP, SSM_STATE), (2.0 * SSM_STATE) ** -0.5),
        "ssm_D": 1.0 + nrm(ks[16], (L, SSM_GROUPS, SSM_GROUP), 0.1),
        "w_glu": nrm(ks[17], (L, SSM_WIDTH, SSM_WIDTH), SSM_WIDTH ** -0.5),
        "b_glu": nrm(ks[18], (L, SSM_WIDTH), 0.01),
        "w_ssm_up": nrm(ks[19], (L, SSM_WIDTH, D_MODEL), SSM_WIDTH ** -0.5),
        "w_attn_up": nrm(ks[20], (L, ATTN_WIDTH, D_MODEL), ATTN_WIDTH ** -0.5),
        "w_out": nrm(ks[21], (L, D_MODEL, D_MODEL), D_MODEL ** -0.5),
        "norm2_w": 1.0 + nrm(ks[22], (L, D_MODEL), 0.02),
        "w_peer_q": nrm(ks[23], (L, D_MODEL, PEER_HEADS * PEER_QUERY_DIM), D_MODEL ** -0.5),
        "peer_sub_keys": nrm(ks[24], (L, PEER_HEADS, 2, PEER_N_KEYS, PEER_HALF_DIM), PEER_HALF_DIM ** -0.5),
        "peer_u": nrm(ks[25], (L, PEER_N_EXPERTS, D_MODEL), D_MODEL ** -0.5),
        "peer_v": nrm(ks[26], (L, PEER_N_EXPERTS, D_MODEL), 0.25),
    }


def reference(x, c, w_ada, b_ada, norm1_w, w_in, b_forget, q_norm_w, k_norm_w,
              ssm_A_re, ssm_A_im, ssm_log_dt, ssm_B_re, ssm_B_im, ssm_C_re, ssm_C_im, ssm_D,
              w_glu, b_glu, w_ssm_up, w_attn_up, w_out, norm2_w, w_peer_q, peer_sub_keys,
              peer_u, peer_v):
    cond = jax.nn.silu(c)
    for l in range(DEPTH):
        mod = cond @ w_ada[l] + b_ada[l]
        sh1, sc1, g1, sh2, sc2, g2 = jnp.split(mod, N_MOD, axis=-1)
        h = modulate(rms_norm(x, norm1_w[l]), sh1, sc1)
        proj = h @ w_in[l]
        q, k, v, f_logit, u, gate_s, gate_a = jnp.split(proj, IN_SPLIT_POINTS, axis=-1)
        y_ssm = s5_mixer(u, ssm_A_re[l], ssm_A_im[l], ssm_log_dt[l], ssm_B_re[l], ssm_B_im[l],
                         ssm_C_re[l], ssm_C_im[l], ssm_D[l])
        y_ssm = half_glu(y_ssm, w_glu[l], b_glu[l])
        y_attn = fox_attention(q, k, v, f_logit, b_forget[l], q_norm_w[l], k_norm_w[l])
        merged = (jax.nn.sigmoid(gate_s) * (y_ssm @ w_ssm_up[l])
                  + jax.nn.sigmoid(gate_a) * (y_attn @ w_attn_up[l]))
        x = x + g1[:, None, :] * (merged @ w_out[l])
        h2 = modulate(rms_norm(x, norm2_w[l]), sh2, sc2)
        x = x + g2[:, None, :] * peer_ffn(h2, w_peer_q[l], peer_sub_keys[l], peer_u[l], peer_v[l])
    return x
```

```python
import contextlib
import numpy as np
import concourse.bass as bass
import concourse.mybir as mybir
from concourse.bass_utils import run_bass_kernel_spmd

F32 = mybir.dt.float32
BF16 = mybir.dt.bfloat16
ALU = mybir.AluOpType
AF = mybir.ActivationFunctionType
AX = mybir.AxisListType

D = 2048
SEQ = 16384
T = 512
NPOS = 32
NSUB = 4
H = 8
GE = 8
NG = 128 // GE
EPS = 1e-6
PADF = 300.0


class Buf:
    __slots__ = ("wf", "wp", "r")

    def __init__(self):
        self.wf = {}; self.wp = {}; self.r = {}


class Sched:
    SAME = True
    ROT = 30000
    NDMA = 16

    def __init__(self, nc, es):
        self.nc = nc; self.es = es
        self.eng = {"pe": nc.tensor, "act": nc.scalar, "dve": nc.vector, "pool": nc.gpsimd, "sp": nc.sync}
        self.nsem = 0
        self.sem = {}; self.cnt = {}; self.waited = {e: {} for e in self.eng}
        for e in self.eng:
            self.sem[e] = self._newsem(); self.cnt[e] = 0
        self.dsem = [self._newsem() for _ in range(self.NDMA)]
        self.dcnt = [0] * self.NDMA
        self.dnext = 0; self.pnext = 0
        self.alltok = {}
        self.ninst = 0

    def _newsem(self):
        self.nsem += 1
        return self.es.enter_context(self.nc.semaphore("sm%d" % self.nsem))

    def _wait(self, e, toks):
        w = self.waited[e]
        best = {}
        for (s, v) in toks:
            if (not self.SAME) and s is self.sem[e]:
                continue
            k = id(s)
            if w.get(k, 0) >= v:
                continue
            if k not in best or best[k][1] < v:
                best[k] = (s, v)
        for k, (s, v) in best.items():
            self.eng[e].wait_ge(s, v)
            w[k] = v

    @staticmethod
    def _put(d, tok):
        k = id(tok[0])
        if k not in d or d[k][1] < tok[1]:
            d[k] = tok

    def _deps(self, reads, writes, pwrites):
        toks = []
        for b in reads:
            toks.extend(b.wf.values()); toks.extend(b.wp.values())
        for b in writes:
            toks.extend(b.wf.values()); toks.extend(b.wp.values()); toks.extend(b.r.values())
        for b in pwrites:
            toks.extend(b.wf.values()); toks.extend(b.r.values())
        return toks

    def _commit(self, tok, reads, writes, pwrites):
        for b in writes:
            b.wf = {id(tok[0]): tok}; b.wp = {}; b.r = {}
        for b in pwrites:
            if b.r:
                b.wf = {}; b.wp = {}; b.r = {}
            self._put(b.wp, tok)
        for b in reads:
            self._put(b.r, tok)
        self.alltok[id(tok[0])] = tok

    dead = False

    def op(self, e, fn, reads=(), writes=(), pwrites=()):
        if self.dead:
            return None
        self._wait(e, self._deps(reads, writes, pwrites))
        if self.cnt[e] >= self.ROT:
            self.sem[e] = self._newsem(); self.cnt[e] = 0
        inst = fn(self.eng[e])
        self.cnt[e] += 1; self.ninst += 1
        inst.then_inc(self.sem[e], 1)
        tok = (self.sem[e], self.cnt[e])
        self._commit(tok, reads, writes, pwrites)
        return tok

    def dma(self, q, out, in_, reads=(), writes=(), pwrites=()):
        if q == "pool":
            i = self.NDMA - 2 + self.pnext; self.pnext = (self.pnext + 1) % 2
        else:
            i = self.dnext; self.dnext = (self.dnext + 1) % (self.NDMA - 2)
        if self.dead:
            return None
        toks = self._deps(reads, writes, pwrites)
        if self.dcnt[i] > 0:
            toks.append((self.dsem[i], 16 * self.dcnt[i]))
        self._wait(q, toks)
        inst = self.eng[q].dma_start(out=out, in_=in_)
        self.dcnt[i] += 1; self.ninst += 1
        inst.then_inc(self.dsem[i], 16)
        tok = (self.dsem[i], 16 * self.dcnt[i])
        self._commit(tok, reads, writes, pwrites)
        return tok

    def barrier(self):
        if self.dead:
            return
        toks = list(self.alltok.values())
        for e in self.eng:
            self._wait(e, toks)


class TT:
    def __init__(self, t):
        self.t = t; self.b = Buf()

    def __getitem__(self, k):
        return self.t[k]


class _Stop(Exception):
    pass


EVAC = [1]


def build(npos=NPOS, dbg=(), stop=None, fast=False):
    nc = bass.Bass("TRN2", target_bir_lowering=False)
    dbg = set(dbg)
    nown = npos // 4

    def din(name, shape, dt=F32):
        return nc.dram_tensor(name, list(shape), dt, kind="ExternalInput").ap()

    x_in = din("x", [NPOS * T, D])
    c_in = din("c", [128, 16])
    wada_in = din("w_ada", [D, 6 * D])
    bada_in = din("b_ada", [128, 96])
    n1_in = din("norm1_w", [128, 16])
    n2_in = din("norm2_w", [128, 16])
    win_in = din("w_in", [D, 7688])
    negbf_in = din("negbf", [8, 1])
    qw_in = din("qw_bc", [128, 512])
    kw_in = din("kw_bc", [128, 512])
    are_in = din("a_re", [128, 16]); aim_in = din("a_im", [128, 16]); ldt_in = din("log_dt", [128, 16])
    bre_in = din("b_re_pl", [128, 16, 128]); bim_in = din("b_im_pl", [128, 16, 128])
    cre_in = din("c_re_pl", [128, 16, 128]); cim_in = din("c_im_pl", [128, 16, 128])
    dsk_in = din("d_skip", [128, 4])
    wglu_in = din("w_glu", [512, 512]); bglu_in = din("b_glu", [128, 4])
    wsu_in = din("w_ssm_up", [512, D]); wau_in = din("w_attn_up", [1024, D]); wout_in = din("w_out", [D, D])
    wq_in = din("w_peer_q", [D, D])
    keys_in = din("keysT", [128, 16, 128])
    pu_in = din("peer_uT", [128 * 128, 2048])
    pv_in = din("peer_vL", [NG * 16 * 128, GE * 128])
    ident_in = din("ident", [128, 128])
    mask_in = din("maskT", [128, 128])
    fpad_in = din("fpad", [8, NPOS * T])
    keep_in = din("keep", [128, NPOS * NSUB])
    out_t = nc.dram_tensor("out", [NPOS // 4 * T, D], F32, kind="ExternalOutput").ap()
    dbg_out = {}
    def dbgt(name, shape):
        if name in dbg:
            dbg_out[name] = nc.dram_tensor("dbg_" + name, list(shape), F32, kind="ExternalOutput").ap()

    def dscr(name, shape, dt=BF16):
        return TT(nc.dram_tensor(name, list(shape), dt).ap())
    wp1 = dscr("wp1", [7, 128, 16, 512])
    wm = dscr("wm", [16, 128, 44, 128])
    wo = dscr("wo", [16, 128, 16, 128])
    wqs = dscr("wqs", [16, 128, 16, 128])
    wgl = dscr("wgl", [4, 128, 4, 128])
    pus = dscr("pus", [128 * 128, 2048])
    pvs = dscr("pvs", [NG * 16 * 128, GE * 128])
    kts = dscr("kts", [128, H, NPOS * T])
    vts = dscr("vts", [128, H, NPOS * NSUB, 128])
    ssmw = dscr("ssmw", [128, 4, 16, 128])
    ssmt = dscr("ssmt", [128, 2, 16, 128], F32)

    with contextlib.ExitStack() as es:
        S = Sched(nc, es)

        uid = [0]

        def sb(st, name, shape, dt=F32):
            uid[0] += 1
            return TT(st.enter_context(nc.sbuf_tensor("%s_%d" % (name, uid[0]), list(shape), dt)))

        def ps(st, name, shape, dt=F32):
            uid[0] += 1
            return TT(st.enter_context(nc.psum_tensor("%s_%d" % (name, uid[0]), list(shape), dt)))

        def dve(fn, r=(), w=(), pw=()):
            return S.op("dve", fn, [t.b for t in r], [t.b for t in w], [t.b for t in pw])

        def act(fn, r=(), w=(), pw=()):
            return S.op("act", fn, [t.b for t in r], [t.b for t in w], [t.b for t in pw])

        def pool(fn, r=(), w=(), pw=()):
            return S.op("pool", fn, [t.b for t in r], [t.b for t in w], [t.b for t in pw])

        pool = dve

        def pe(fn, r=(), w=(), pw=()):
            return S.op("pe", fn, [t.b for t in r], [t.b for t in w], [t.b for t in pw])

        def dma(out, in_, r=(), w=(), pw=(), q="sp"):
            return S.dma(q, out, in_, [t.b for t in r], [t.b for t in w], [t.b for t in pw])

        def dump(name, ap, r):
            if name in dbg_out:
                dma(dbg_out[name], ap, r=r)

        def ckpt(name):
            if stop == name:
                S.barrier()
                S.dead = True

        identf = sb(es, "identf", [128, 128]); identb = sb(es, "identb", [128, 128], BF16)
        maskT = sb(es, "maskT", [128, 128])
        selh = sb(es, "selh", [8, 128]); negI = sb(es, "negI", [8, 8])
        sc1 = sb(es, "sc1", [128, 16]); bi1 = sb(es, "bi1", [128, 16]); g1 = sb(es, "g1", [128, 16])
        sc2 = sb(es, "sc2", [128, 16]); bi2 = sb(es, "bi2", [128, 16]); g2 = sb(es, "g2", [128, 16])
        qwb = sb(es, "qwb", [128, 512]); kwb = sb(es, "kwb", [128, 512])
        wfb = sb(es, "wfb", [128, 16, 8], BF16)
        negbf = sb(es, "negbf", [8, 1])
        mag = sb(es, "mag", [128, 16]); Rre = sb(es, "Rre", [128, 16]); Rim = sb(es, "Rim", [128, 16])
        wend = sb(es, "wend", [128, 16, 2])
        dsk = sb(es, "dsk", [128, 4]); bglu = sb(es, "bglu", [128, 4])
        negFk = sb(es, "negFk", [128, NPOS * NSUB, 8])
        negFT = sb(es, "negFT", [8, T]); fcar = sb(es, "fcar", [8, 1])
        keep = sb(es, "keep", [128, NPOS * NSUB])
        onesT = sb(es, "onesT", [128, T])

        cdma = lambda dst, src: dma(dst.t[:], src, w=[dst])
        cdma(identf, ident_in[:, :]); cdma(maskT, mask_in[:, :])
        cdma(qwb, qw_in[:, :]); cdma(kwb, kw_in[:, :]); cdma(negbf, negbf_in[:, :])
        cdma(dsk, dsk_in[:, :]); cdma(bglu, bglu_in[:, :]); cdma(keep, keep_in[:, :])
        dve(lambda e: e.tensor_copy(identb[:], identf[:]), r=[identf], w=[identb])
        dve(lambda e: e.tensor_scalar(negI[:], identf[0:8, 0:8], -1.0, None, ALU.mult), r=[identf], w=[negI])
        dve(lambda e: e.tensor_scalar(negbf[:], negbf[:], -1.0, None, ALU.mult), r=[negbf], w=[negbf])
        dve(lambda e: e.tensor_scalar(qwb[:], qwb[:], float(128 ** -0.5), None, ALU.mult), r=[qwb], w=[qwb])
        dve(lambda e: e.memset(onesT[:], 1.0), w=[onesT])
        dve(lambda e: e.memset(wend[:], 0.0), w=[wend])
        dve(lambda e: e.memset(fcar[:], 0.0), w=[fcar])

        winv = win_in.rearrange("(c p) n -> p c n", p=128)
        wsuv = wsu_in.rearrange("(c p) n -> p c n", p=128)
        wauv = wau_in.rearrange("(c p) n -> p c n", p=128)
        woutv = wout_in.rearrange("(c p) n -> p c n", p=128)
        wqv = wq_in.rearrange("(c p) n -> p c n", p=128)
        wgluv = wglu_in.rearrange("(c p) n -> p c n", p=128)
        keysb = sb(es, "keysb", [128, 16, 128], BF16)
        cvk = [0]

        def convert_all(parts):
            with contextlib.ExitStack() as st:
                stg = [sb(st, "stg%d" % i, [128, 8192]) for i in range(2)]
                stb = [sb(st, "stb%d" % i, [128, 8192], BF16) for i in range(2)]
                for (loads, n_el, stores) in parts:
                    k = cvk[0]; cvk[0] += 1
                    s32 = stg[k % 2]; s16 = stb[k % 2]
                    for (off, shp, src_ap) in loads:
                        dst = s32[:, off:off + int(np.prod(shp))]
                        if len(shp) == 2:
                            dst = dst.rearrange("p (a b) -> p a b", a=shp[0])
                        dma(dst, src_ap, pw=[s32])
                    eng = (act, dve, pool)[k % 3]
                    if eng is act:
                        act(lambda e: e.activation(out=s16[:, 0:n_el], in_=s32[:, 0:n_el], func=AF.Copy), r=[s32], w=[s16])
                    else:
                        eng(lambda e: e.tensor_copy(s16[:, 0:n_el], s32[:, 0:n_el]), r=[s32], w=[s16])
                    for (off, shp, dst_ap, dtt, *insb) in stores:
                        s_ = s16[:, off:off + int(np.prod(shp))]
                        if len(shp) == 2:
                            s_ = s_.rearrange("p (a b) -> p a b", a=shp[0])
                        if insb:
                            dve(lambda e: e.tensor_copy(dst_ap, s_), r=[s16], w=[dtt])
                        else:
                            dma(dst_ap, s_, r=[s16], pw=[dtt])
                S.barrier()

        def parts_first():
            parts = []
            pcols = [1024, 1536, 2048, 2560, 3080, 0, 512]
            for i, c0 in enumerate(pcols):
                parts.append(([(0, (16, 512), winv[:, :, c0:c0 + 512])], 8192, [(0, (16, 512), wp1.t[i], wp1)]))
            parts.append(([(0, (16, 8), winv[:, :, 3072:3080])], 128, [(0, (16, 8), wfb[:], wfb, True)]))
            parts.append(([(0, (16, 128), keys_in[:, :, :])], 2048, [(0, (16, 128), keysb[:], keysb, True)]))
            return parts

        def parts_rest():
            parts = []
            for n in range(4):
                parts.append(([(0, (4, 128), wgluv[:, :, n * 128:(n + 1) * 128])], 512, [(0, (4, 128), wgl.t[n], wgl)]))
            for n in range(16):
                ns = slice(n * 128, (n + 1) * 128)
                parts.append(([(0, (4, 128), wsuv[:, :, ns]), (512, (8, 128), wauv[:, :, ns]),
                               (1536, (16, 128), winv[:, :, 3592 + n * 128:3592 + (n + 1) * 128]),
                               (3584, (16, 128), winv[:, :, 5640 + n * 128:5640 + (n + 1) * 128])], 5632,
                              [(0, (44, 128), wm.t[n], wm)]))
                parts.append(([(0, (16, 128), woutv[:, :, ns]), (2048, (16, 128), wqv[:, :, ns])], 4096,
                              [(0, (16, 128), wo.t[n], wo), (2048, (16, 128), wqs.t[n], wqs)]))
            for (src_, dst_, nrows, ncols) in ((pu_in, pus, 128 * 128, 2048), (pv_in, pvs, NG * 16 * 128, GE * 128)):
                rr = 8192 // ncols
                for k in range(nrows // (128 * rr)):
                    rs_ = slice(k * 128 * rr, (k + 1) * 128 * rr)
                    parts.append(([(0, (8192,), src_[rs_, :].rearrange("(p r) c -> p (r c)", r=rr))], 8192,
                                  [(0, (8192,), dst_.t[rs_, :].rearrange("(p r) c -> p (r c)", r=rr), dst_)]))
            return parts

        if not fast:
            convert_all(parts_first())

        def conv_rest():
            convert_all(parts_rest())

        ckpt("consts")
        if fast:
            for t_ in (sc1, bi1, g1, sc2, bi2, g2):
                dve(lambda e, t_=t_: e.memset(t_[:], 0.5), w=[t_])
        with contextlib.ExitStack() as st:
          if not fast:
            cnd = sb(st, "cnd", [128, 16]); n1 = sb(st, "n1", [128, 16]); n2 = sb(st, "n2", [128, 16])
            modv = sb(st, "modv", [128, 96]); bad = sb(st, "bad", [128, 96])
            wa = [sb(st, "wa%d" % i, [128, 6144]) for i in range(2)]
            pm = [ps(st, "pm%d" % i, [128, 512]) for i in range(2)]
            cdma(cnd, c_in[:, :]); cdma(n1, n1_in[:, :]); cdma(n2, n2_in[:, :]); cdma(bad, bada_in[:, :])
            act(lambda e: e.activation(out=cnd[:], in_=cnd[:], func=AF.Silu), r=[cnd], w=[cnd])
            dve(lambda e: e.tensor_copy(modv[:], bad[:]), r=[bad], w=[modv])
            k = 0
            for kc in range(16):
                for hf in range(2):
                    wb = wa[k % 2]; pb = pm[k % 2]; k += 1
                    dma(wb.t[:], wada_in[kc * 128:(kc + 1) * 128, hf * 6144:(hf + 1) * 6144], w=[wb])
                    for n in range(48):
                        pe(lambda e, n=n, wb=wb, pb=pb, kc=kc: e.matmul(pb[:, n:n + 1], lhsT=wb[:, n * 128:(n + 1) * 128],
                                                                        rhs=cnd[:, kc:kc + 1], start=True, stop=True),
                           r=[wb, cnd], pw=[pb])
                    dve(lambda e, hf=hf, pb=pb: e.tensor_tensor(out=modv[:, hf * 48:(hf + 1) * 48], in0=modv[:, hf * 48:(hf + 1) * 48],
                                                                in1=pb[:, 0:48], op=ALU.add), r=[pb], w=[modv])
            for (scx, bix, gx, nx, o) in ((sc1, bi1, g1, n1, 0), (sc2, bi2, g2, n2, 48)):
                dve(lambda e, scx=scx, nx=nx, o=o: e.scalar_tensor_tensor(out=scx[:], in0=modv[:, o + 16:o + 32], scalar=1.0, in1=nx[:],
                                                                         op0=ALU.add, op1=ALU.mult), r=[modv, nx], w=[scx])
                dve(lambda e, bix=bix, o=o: e.tensor_copy(bix[:], modv[:, o:o + 16]), r=[modv], w=[bix])
                dve(lambda e, gx=gx, o=o: e.tensor_copy(gx[:], modv[:, o + 32:o + 48]), r=[modv], w=[gx])
            if "mod" in dbg:
                dbgt("mod", [128, 96]); dump("mod", modv[:], [modv])
            S.barrier()

        ckpt("mod")
        with contextlib.ExitStack() as st:
          if not fast:
            P = lambda n: sb(st, n, [128, 16])
            are = P("are"); aim = P("aim"); dt_ = P("dt"); th = P("th"); t0 = P("t0"); t1 = P("t1"); t2 = P("t2")
            sn = P("sn"); cs = P("cs"); abr = P("abr"); abi = P("abi"); cfr = P("cfr"); cfi = P("cfi"); den = P("den")
            cdma(are, are_in[:, :]); cdma(aim, aim_in[:, :]); cdma(dt_, ldt_in[:, :])
            act(lambda e: e.activation(out=dt_[:], in_=dt_[:], func=AF.Exp), r=[dt_], w=[dt_])
            dve(lambda e: e.tensor_tensor(out=t0[:], in0=are[:], in1=dt_[:], op=ALU.mult), r=[are, dt_], w=[t0])
            act(lambda e: e.activation(out=mag[:], in_=t0[:], func=AF.Exp), r=[t0], w=[mag])
            dve(lambda e: e.tensor_tensor(out=th[:], in0=aim[:], in1=dt_[:], op=ALU.mult), r=[aim, dt_], w=[th])
            TWO_PI = 2.0 * np.pi
            for kk in (16.0, 8.0, 4.0, 2.0, 1.0):
                dve(lambda e, kk=kk: e.tensor_scalar(t0[:], th[:], float(kk * TWO_PI - np.pi), float(-kk * TWO_PI), ALU.is_gt, ALU.mult),
                    r=[th], w=[t0])
                dve(lambda e: e.tensor_tensor(out=th[:], in0=th[:], in1=t0[:], op=ALU.add), r=[th, t0], w=[th])
            for kk in (1.0,):
                dve(lambda e, kk=kk: e.tensor_scalar(t0[:], th[:], float(-np.pi), float(TWO_PI), ALU.is_lt, ALU.mult), r=[th], w=[t0])
                dve(lambda e: e.tensor_tensor(out=th[:], in0=th[:], in1=t0[:], op=ALU.add), r=[th, t0], w=[th])
            dve(lambda e: e.tensor_scalar(t0[:], th[:], 0.125, None, ALU.mult), r=[th], w=[t0])
            dve(lambda e: e.tensor_tensor(out=t1[:], in0=t0[:], in1=t0[:], op=ALU.mult), r=[t0], w=[t1])
            dve(lambda e: e.tensor_scalar(sn[:], t1[:], -1.0 / 72.0, 1.0, ALU.mult, ALU.add), r=[t1], w=[sn])
            for cst in (42.0, 20.0, 6.0):
                dve(lambda e: e.tensor_tensor(out=sn[:], in0=sn[:], in1=t1[:], op=ALU.mult), r=[sn, t1], w=[sn])
                dve(lambda e, cst=cst: e.tensor_scalar(sn[:], sn[:], -1.0 / cst, 1.0, ALU.mult, ALU.add), r=[sn], w=[sn])
            dve(lambda e: e.tensor_tensor(out=sn[:], in0=sn[:], in1=t0[:], op=ALU.mult), r=[sn, t0], w=[sn])
            dve(lambda e: e.tensor_scalar(cs[:], t1[:], -1.0 / 56.0, 1.0, ALU.mult, ALU.add), r=[t1], w=[cs])
            for cst in (30.0, 12.0, 2.0):
                dve(lambda e: e.tensor_tensor(out=cs[:], in0=cs[:], in1=t1[:], op=ALU.mult), r=[cs, t1], w=[cs])
                dve(lambda e, cst=cst: e.tensor_scalar(cs[:], cs[:], -1.0 / cst, 1.0, ALU.mult, ALU.add), r=[cs], w=[cs])

            def cdouble(cr, ci):
                dve(lambda e: e.tensor_tensor(out=t1[:], in0=cr[:], in1=ci[:], op=ALU.mult), r=[cr, ci], w=[t1])
                dve(lambda e: e.tensor_tensor(out=t2[:], in0=ci[:], in1=ci[:], op=ALU.mult), r=[ci], w=[t2])
                dve(lambda e: e.tensor_tensor(out=cr[:], in0=cr[:], in1=cr[:], op=ALU.mult), r=[cr], w=[cr])
                dve(lambda e: e.tensor_tensor(out=cr[:], in0=cr[:], in1=t2[:], op=ALU.subtract), r=[cr, t2], w=[cr])
                dve(lambda e: e.tensor_scalar(ci[:], t1[:], 2.0, None, ALU.mult), r=[t1], w=[ci])
            for _ in range(3):
                cdouble(cs, sn)
            dve(lambda e: e.tensor_tensor(out=abr[:], in0=mag[:], in1=cs[:], op=ALU.mult), r=[mag, cs], w=[abr])
            dve(lambda e: e.tensor_tensor(out=abi[:], in0=mag[:], in1=sn[:], op=ALU.mult), r=[mag, sn], w=[abi])
            dve(lambda e: e.tensor_tensor(out=den[:], in0=are[:], in1=are[:], op=ALU.mult), r=[are], w=[den])
            dve(lambda e: e.tensor_tensor(out=t0[:], in0=aim[:], in1=aim[:], op=ALU.mult), r=[aim], w=[t0])
            dve(lambda e: e.tensor_tensor(out=den[:], in0=den[:], in1=t0[:], op=ALU.add), r=[den, t0], w=[den])
            dve(lambda e: e.reciprocal(den[:], den[:]), r=[den], w=[den])
            dve(lambda e: e.tensor_scalar(t0[:], abr[:], -1.0, None, ALU.add), r=[abr], w=[t0])
            dve(lambda e: e.tensor_tensor(out=cfr[:], in0=t0[:], in1=are[:], op=ALU.mult), r=[t0, are], w=[cfr])
            dve(lambda e: e.tensor_tensor(out=t1[:], in0=abi[:], in1=aim[:], op=ALU.mult), r=[abi, aim], w=[t1])
            dve(lambda e: e.tensor_tensor(out=cfr[:], in0=cfr[:], in1=t1[:], op=ALU.add), r=[cfr, t1], w=[cfr])
            dve(lambda e: e.tensor_tensor(out=cfr[:], in0=cfr[:], in1=den[:], op=ALU.mult), r=[cfr, den], w=[cfr])
            dve(lambda e: e.tensor_tensor(out=cfi[:], in0=abi[:], in1=are[:], op=ALU.mult), r=[abi, are], w=[cfi])
            dve(lambda e: e.tensor_tensor(out=t1[:], in0=t0[:], in1=aim[:], op=ALU.mult), r=[t0, aim], w=[t1])
            dve(lambda e: e.tensor_tensor(out=cfi[:], in0=cfi[:], in1=t1[:], op=ALU.subtract), r=[cfi, t1], w=[cfi])
            dve(lambda e: e.tensor_tensor(out=cfi[:], in0=cfi[:], in1=den[:], op=ALU.mult), r=[cfi, den], w=[cfi])
            rc = sb(st, "rc", [128, 16, 128]); rsn = sb(st, "rsn", [128, 16, 128])
            pr = P("pr"); pi_ = P("pi")
            dve(lambda e: e.tensor_copy(pr[:], cs[:]), r=[cs], w=[pr])
            dve(lambda e: e.tensor_copy(pi_[:], sn[:]), r=[sn], w=[pi_])
            dve(lambda e: e.memset(rc[:, :, 0:1], 1.0), w=[rc])
            dve(lambda e: e.memset(rsn[:, :, 0:1], 0.0), w=[rsn])
            tmpa = sb(st, "tmpa", [128, 16, 64]); tmpb = sb(st, "tmpb", [128, 16, 64])
            n_ = 1
            while n_ < 128:
                bc = lambda tt_: tt_[:].unsqueeze(2).to_broadcast([128, 16, n_])
                lo = slice(0, n_); hi = slice(n_, 2 * n_)
                dve(lambda e, bc=bc, lo=lo: e.tensor_tensor(out=tmpa[:, :, lo], in0=rc[:, :, lo], in1=bc(pr), op=ALU.mult), r=[rc, pr], w=[tmpa])
                dve(lambda e, bc=bc, lo=lo: e.tensor_tensor(out=tmpb[:, :, lo], in0=rsn[:, :, lo], in1=bc(pi_), op=ALU.mult), r=[rsn, pi_], w=[tmpb])
                dve(lambda e, lo=lo, hi=hi: e.tensor_tensor(out=rc[:, :, hi], in0=tmpa[:, :, lo], in1=tmpb[:, :, lo], op=ALU.subtract), r=[tmpa, tmpb, rsn], w=[rc])
                dve(lambda e, bc=bc, lo=lo: e.tensor_tensor(out=tmpa[:, :, lo], in0=rc[:, :, lo], in1=bc(pi_), op=ALU.mult), r=[rc, pi_], w=[tmpa])
                dve(lambda e, bc=bc, lo=lo: e.tensor_tensor(out=tmpb[:, :, lo], in0=rsn[:, :, lo], in1=bc(pr), op=ALU.mult), r=[rsn, pr], w=[tmpb])
                dve(lambda e, lo=lo, hi=hi: e.tensor_tensor(out=rsn[:, :, hi], in0=tmpa[:, :, lo], in1=tmpb[:, :, lo], op=ALU.add), r=[tmpa, tmpb, rc], w=[rsn])
                cdouble(pr, pi_)
                n_ *= 2
            dve(lambda e: e.tensor_copy(Rre[:], pr[:]), r=[pr], w=[Rre])
            dve(lambda e: e.tensor_copy(Rim[:], pi_[:]), r=[pi_], w=[Rim])
            dma(ssmt.t[:, 0], rc[:], r=[rc], pw=[ssmt]); dma(ssmt.t[:, 1], rsn[:], r=[rsn], pw=[ssmt])
            bre = sb(st, "bre", [128, 16, 128]); bim = sb(st, "bim", [128, 16, 128])
            mre = sb(st, "mre", [128, 16, 128]); mim = sb(st, "mim", [128, 16, 128]); mt = sb(st, "mt", [128, 16, 128])
            cdma(bre, bre_in[:, :, :]); cdma(bim, bim_in[:, :, :])
            bcc = lambda tt_: tt_[:].unsqueeze(2).to_broadcast([128, 16, 128])
            dve(lambda e: e.tensor_tensor(out=mre[:], in0=bre[:], in1=bcc(cfr), op=ALU.mult), r=[bre, cfr], w=[mre])
            dve(lambda e: e.tensor_tensor(out=mt[:], in0=bim[:], in1=bcc(cfi), op=ALU.mult), r=[bim, cfi], w=[mt])
            dve(lambda e: e.tensor_tensor(out=mre[:], in0=mre[:], in1=mt[:], op=ALU.subtract), r=[mre, mt], w=[mre])
            dve(lambda e: e.tensor_tensor(out=mim[:], in0=bim[:], in1=bcc(cfr), op=ALU.mult), r=[bim, cfr], w=[mim])
            dve(lambda e: e.tensor_tensor(out=mt[:], in0=bre[:], in1=bcc(cfi), op=ALU.mult), r=[bre, cfi], w=[mt])
            dve(lambda e: e.tensor_tensor(out=mim[:], in0=mim[:], in1=mt[:], op=ALU.add), r=[mim, mt], w=[mim])
            sw = sb(st, "sw", [128, 4, 16, 128], BF16)
            ptp = [ps(st, "ptp%d" % i, [128, 4, 128]) for i in range(2)]
            k = 0
            for mi, msrc in enumerate((mre, mim)):
                for q4 in range(4):
                    pb = ptp[k % 2]; k += 1
                    for a in range(4):
                        pe(lambda e, a=a, q4=q4, msrc=msrc, pb=pb: e.transpose(out=pb[:, a, :], in_=msrc[:, q4 * 4 + a, :], identity=identf[:]),
                           r=[msrc, identf], pw=[pb])
                    act(lambda e, mi=mi, q4=q4, pb=pb: e.activation(out=sw[:, mi, q4 * 4:(q4 + 1) * 4, :], in_=pb[:], func=AF.Copy), r=[pb], pw=[sw])
            cdma(bre, cre_in[:, :, :]); cdma(bim, cim_in[:, :, :])
            dve(lambda e: e.tensor_copy(sw[:, 2], bre[:]), r=[bre], pw=[sw])
            dve(lambda e: e.tensor_scalar(sw[:, 3], bim[:], -1.0, None, ALU.mult), r=[bim], pw=[sw])
            dma(ssmw.t[:], sw[:], r=[sw], w=[ssmw])
            if "ssmp" in dbg:
                dbgt("ssmp", [128, 6, 16])
                for i_, tt_ in enumerate((abr, abi, cfr, cfi, Rre, Rim)):
                    dump("ssmp", None, None) if False else dma(dbg_out["ssmp"][:, i_, :], tt_[:], r=[tt_])
            S.barrier()

        ckpt("ssmp")
        if not fast:
            conv_rest()
        ckpt("conv")

        def rmsnorm_to_fm(st, src_tok, scx, bix, hT, nm):
            junk = sb(st, nm + "junk", [128, D], BF16)
            ss = sb(st, nm + "ss", [128, NSUB])
            xn = [sb(st, nm + "xn%d" % i, [128, D]) for i in range(2)]
            ptr = [ps(st, nm + "ptr%d" % i, [128, 4, 128]) for i in range(2)]
            k = 0
            for sub in range(NSUB):
                ap, tt_ = src_tok(sub)
                act(lambda e, ap=ap, sub=sub: e.activation(out=junk[:], in_=ap, func=AF.Square, accum_out=ss[:, sub:sub + 1]),
                    r=[tt_], w=[junk], pw=[ss])
                act(lambda e, sub=sub: e.activation(out=ss[:, sub:sub + 1], in_=ss[:, sub:sub + 1], func=AF.Sqrt, bias=EPS, scale=1.0 / D),
                    r=[ss], pw=[ss])
                dve(lambda e, sub=sub: e.reciprocal(ss[:, sub:sub + 1], ss[:, sub:sub + 1]), r=[ss], pw=[ss])
                ckpt("%s_a%d_%d" % (nm, sub, uid[0] * 0))
                xb = xn[sub % 2]
                pool(lambda e, ap=ap, sub=sub, xb=xb: e.tensor_scalar(xb[:], ap, ss[:, sub:sub + 1], None, ALU.mult), r=[tt_, ss], w=[xb])
                ckpt("%s_b%d_0" % (nm, sub))
                for q4 in range(4):
                    ckpt("%s_e%d_%d" % (nm, sub, q4))
                    pb = ptr[k % 2]; k += 1
                    for a in range(4):
                        dc = q4 * 4 + a
                        pe(lambda e, a=a, dc=dc, xb=xb, pb=pb: e.transpose(out=pb[:, a, :], in_=xb[:, dc * 128:(dc + 1) * 128], identity=identf[:]),
                           r=[xb, identf], pw=[pb])
                    ckpt("%s_t%d_%d" % (nm, sub, q4))
                    for a in range(4):
                        dc = q4 * 4 + a
                        o_ = hT[:, dc, sub * 128:(sub + 1) * 128]
                        if (a % 2 == 0 and EVAC[0] == 2) or EVAC[0] == 1:
                            act(lambda e, o_=o_, a=a, dc=dc, pb=pb: e.activation(out=o_, in_=pb[:, a, :], func=AF.Identity,
                                                                               bias=bix[:, dc:dc + 1], scale=scx[:, dc:dc + 1]),
                                r=[pb, bix, scx], pw=[hT])
                        else:
                            dve(lambda e, o_=o_, a=a, dc=dc, pb=pb: e.tensor_scalar(o_, pb[:, a, :], scx[:, dc:dc + 1], bix[:, dc:dc + 1], ALU.mult, ALU.add),
                                r=[pb, bix, scx], pw=[hT])

        def headnorm_T(st, hT, widx, wbc, dstT, nm, pools):
            wpc, pkv, ptb, kf, ksq, kss, kb_ = pools
            k = 0
            for half in range(2):
                wb = wpc[widx[half] % 2]
                dma(wb.t[:], wp1.t[widx[half]], r=[wp1], w=[wb])
                for sub in range(NSUB):
                    pb = pkv[k % 2]; k += 1
                    for dc in range(16):
                        pe(lambda e, dc=dc, sub=sub, wb=wb, pb=pb: e.matmul(pb[:], lhsT=hT[:, dc, sub * 128:(sub + 1) * 128], rhs=wb[:, dc, :],
                                                                          start=(dc == 0), stop=(dc == 15)), r=[hT, wb], pw=[pb])
                    act(lambda e, pb=pb: e.activation(out=kf[:], in_=pb[:], func=AF.Copy), r=[pb], w=[kf])
                    dve(lambda e: e.tensor_tensor(out=ksq[:], in0=kf[:], in1=kf[:], op=ALU.mult), r=[kf], w=[ksq])
                    dve(lambda e: e.tensor_reduce(out=kss[:], in_=ksq[:].rearrange("p (h d) -> p h d", h=4), axis=AX.X, op=ALU.add), r=[ksq], w=[kss])
                    act(lambda e: e.activation(out=kss[:], in_=kss[:], func=AF.Sqrt, bias=EPS, scale=1.0 / 128), r=[kss], w=[kss])
                    dve(lambda e: e.reciprocal(kss[:], kss[:]), r=[kss], w=[kss])
                    dve(lambda e: e.tensor_tensor(out=kf[:].rearrange("p (h d) -> p h d", h=4), in0=kf[:].rearrange("p (h d) -> p h d", h=4),
                                                  in1=kss[:].unsqueeze(2).to_broadcast([128, 4, 128]), op=ALU.mult), r=[kf, kss], w=[kf])
                    dve(lambda e: e.tensor_tensor(out=kb_[:], in0=kf[:], in1=wbc[:], op=ALU.mult), r=[kf, wbc], w=[kb_])
                    for a in range(4):
                        pe(lambda e, a=a: e.transpose(out=ptb[:, a, :], in_=kb_[:, a * 128:(a + 1) * 128], identity=identb[:]), r=[kb_, identb], pw=[ptb])
                    act(lambda e, half=half, sub=sub: e.activation(out=dstT[:, half * 4:(half + 1) * 4, sub * 128:(sub + 1) * 128], in_=ptb[:], func=AF.Copy),
                        r=[ptb], pw=[dstT])

        for pos in range(npos):
            own = (pos % 4 == 3)
            oi = pos // 4
            tok0 = pos * T
            x1_st = contextlib.ExitStack()
            x1 = sb(x1_st, "x1", [128, NSUB, D]) if own else None
            tile_st = contextlib.ExitStack()
            if True:
                hT = sb(tile_st, "hT", [128, 16, T], BF16)
                uT = sb(tile_st, "uT", [128, 4, T], BF16)
                u32 = sb(tile_st, "u32", [128, 4, T]) if own else None
                yssmT = sb(tile_st, "yssmT", [128, 4, T], BF16) if own else None
                with contextlib.ExitStack() as st:
                    xs = [sb(st, "xs%d" % i, [128, D]) for i in range(2)]
                    if own:
                        for sub in range(NSUB):
                            dma(x1.t[:, sub, :], x_in[tok0 + sub * 128:tok0 + (sub + 1) * 128, :], pw=[x1])
                        src = lambda sub: (x1[:, sub, :], x1)
                    else:
                        def src(sub):
                            xb = xs[sub % 2]
                            dma(xb.t[:], x_in[tok0 + sub * 128:tok0 + (sub + 1) * 128, :], w=[xb])
                            return xb[:], xb
                    rmsnorm_to_fm(st, src, sc1, bi1, hT, "n1")
                    S.barrier()
                if "hT" in dbg and pos == 3:
                    with contextlib.ExitStack() as st:
                        dbgt("hT", [128, 16, T])
                        hdb = sb(st, "hdb", [128, 16, T])
                        dve(lambda e: e.tensor_copy(hdb[:], hT[:]), r=[hT], w=[hdb])
                        dump("hT", hdb[:], [hdb])
                        S.barrier()
                ckpt("norm%d" % pos)
                with contextlib.ExitStack() as st2:
                    wb = sb(st2, "wu", [128, 16, 512], BF16)
                    pkv = [ps(st2, "pkv%d" % i, [128, 512]) for i in range(2)]
                    pf = ps(st2, "pf", [8, T]); pft = ps(st2, "pft", [128, NSUB, 8])
                    dma(wb.t[:], wp1.t[4], r=[wp1], w=[wb])
                    for uc in range(4):
                        pb = pkv[uc % 2]
                        for dc in range(16):
                            pe(lambda e, dc=dc, uc=uc, pb=pb: e.matmul(pb[:], lhsT=wb[:, dc, uc * 128:(uc + 1) * 128], rhs=hT[:, dc, :],
                                                                      start=(dc == 0), stop=(dc == 15)), r=[hT, wb], pw=[pb])
                        if own:
                            act(lambda e, uc=uc, pb=pb: e.activation(out=u32[:, uc, :], in_=pb[:], func=AF.Copy), r=[pb], pw=[u32])
                            dve(lambda e, uc=uc: e.tensor_copy(uT[:, uc, :], u32[:, uc, :]), r=[u32], pw=[uT])
                        else:
                            act(lambda e, uc=uc, pb=pb: e.activation(out=uT[:, uc, :], in_=pb[:], func=AF.Copy), r=[pb], pw=[uT])
                    lf = sb(st2, "lf", [8, T]); fp_ = sb(st2, "fp_", [8, T])
                    dma(fp_.t[:], fpad_in[:, tok0:tok0 + T], w=[fp_])
                    for dc in range(16):
                        pe(lambda e, dc=dc: e.matmul(pf[:], lhsT=wfb[:, dc, :], rhs=hT[:, dc, :], start=(dc == 0), stop=(dc == 15)), r=[hT, wfb], pw=[pf])
                    act(lambda e: e.activation(out=lf[:], in_=pf[:], func=AF.Exp, bias=negbf[:, 0:1], scale=-1.0), r=[pf, negbf], w=[lf])
                    act(lambda e: e.activation(out=lf[:], in_=lf[:], func=AF.Ln, bias=1.0), r=[lf], w=[lf])
                    dve(lambda e: e.tensor_tensor(out=lf[:], in0=lf[:], in1=fp_[:], op=ALU.add), r=[lf, fp_], w=[lf])
                    dve(lambda e: e.tensor_tensor_scan(out=negFT[:], data0=onesT[0:8, :], data1=lf[:], initial=fcar[:, 0:1], op0=ALU.mult, op1=ALU.add),
                        r=[onesT, lf, fcar], w=[negFT])
                    dve(lambda e: e.tensor_copy(fcar[:], negFT[:, T - 1:T]), r=[negFT], w=[fcar])
                    for sub in range(NSUB):
                        pe(lambda e, sub=sub: e.transpose(out=pft[:, sub, :], in_=negFT[:, sub * 128:(sub + 1) * 128], identity=identf[0:8, 0:8]),
                           r=[negFT, identf], pw=[pft])
                    dve(lambda e: e.tensor_copy(negFk[:, pos * NSUB:(pos + 1) * NSUB, :], pft[:]), r=[pft], pw=[negFk])
                    if "negF" in dbg and pos == 3:
                        dbgt("negF", [8, T]); dump("negF", negFT[:], [negFT])
                    S.barrier()
                if True:
                    ckpt("uf%d" % pos)
                    with contextlib.ExitStack() as st2:
                        sw = sb(st2, "sw_l", [128, 4, 16, 128], BF16); rt = sb(st2, "rt_l", [128, 2, 16, 128])
                        dma(sw.t[:], ssmw.t[:], r=[ssmw], w=[sw]); dma(rt.t[:], ssmt.t[:], r=[ssmt], w=[rt])
                        bu = [sb(st2, "bu%d" % i, [128, 2, T]) for i in range(2)]
                        zz = [sb(st2, "zz%d" % i, [128, 2, T]) for i in range(2)]
                        ta = sb(st2, "ta", [128, T]); tb = sb(st2, "tb", [128, T])
                        ww = [sb(st2, "ww%d" % i, [128, 2, T]) for i in range(2)]
                        ini = sb(st2, "ini", [128, 2]); tin = sb(st2, "tin", [128, 2])
                        magT = sb(st2, "magT", [128, 128])
                        sst = sb(st2, "sst", [128, 4, 2, T], BF16) if own else None
                        y32 = sb(st2, "y32", [128, T]) if own else None
                        g32 = sb(st2, "g32", [128, 4, T]) if own else None
                        gb = sb(st2, "gb", [128, 4, T], BF16) if own else None
                        pbu = [ps(st2, "pbu%d" % i, [128, 2, T]) for i in range(2)]
                        py = ps(st2, "py", [128, T]) if own else None
                        r4 = lambda ap: ap.rearrange("p (s l) -> p s l", s=NSUB)
                        for pc in range(16):
                            uc = pc // 4
                            pb = pbu[pc % 2]; bb = bu[pc % 2]; zb = zz[pc % 2]; wv = ww[pc % 2]
                            for ri in range(2):
                                pe(lambda e, ri=ri, pc=pc, uc=uc, pb=pb: e.matmul(pb[:, ri, :], lhsT=sw[:, ri, pc, :], rhs=uT[:, uc, :], start=True, stop=True),
                                   r=[sw, uT], pw=[pb])
                            act(lambda e, pb=pb, bb=bb: e.activation(out=bb[:], in_=pb[:], func=AF.Copy), r=[pb], w=[bb])
                            rcb = rt[:, 0, pc, :].unsqueeze(1).to_broadcast([128, NSUB, 128])
                            rsb = rt[:, 1, pc, :].unsqueeze(1).to_broadcast([128, NSUB, 128])
                            pool(lambda e, bb=bb, rcb=rcb: e.tensor_tensor(out=r4(ta[:]), in0=r4(bb[:, 0, :]), in1=rcb, op=ALU.mult), r=[bb, rt], w=[ta])
                            pool(lambda e, bb=bb, rsb=rsb: e.tensor_tensor(out=r4(tb[:]), in0=r4(bb[:, 1, :]), in1=rsb, op=ALU.mult), r=[bb, rt], w=[tb])
                            pool(lambda e, zb=zb: e.tensor_tensor(out=zb[:, 0, :], in0=ta[:], in1=tb[:], op=ALU.add), r=[ta, tb], pw=[zb])
                            pool(lambda e, bb=bb, rcb=rcb: e.tensor_tensor(out=r4(ta[:]), in0=r4(bb[:, 1, :]), in1=rcb, op=ALU.mult), r=[bb, rt, zb], w=[ta])
                            pool(lambda e, bb=bb, rsb=rsb: e.tensor_tensor(out=r4(tb[:]), in0=r4(bb[:, 0, :]), in1=rsb, op=ALU.mult), r=[bb, rt, zb], w=[tb])
                            pool(lambda e, zb=zb: e.tensor_tensor(out=zb[:, 1, :], in0=ta[:], in1=tb[:], op=ALU.subtract), r=[ta, tb], pw=[zb])
                            dve(lambda e, pc=pc: e.tensor_copy(magT[:], mag[:, pc:pc + 1].to_broadcast([128, 128])), r=[mag], w=[magT])
                            for sub in range(NSUB):
                                gsub = pos * NSUB + sub
                                prev = wend[:, pc, :] if sub == 0 else None
                                pr_re = wend[:, pc, 0:1] if sub == 0 else wv[:, 0, sub * 128 - 1:sub * 128]
                                pr_im = wend[:, pc, 1:2] if sub == 0 else wv[:, 1, sub * 128 - 1:sub * 128]
                                rd = [wend] if sub == 0 else [wv]
                                dve(lambda e, pr_im=pr_im, pc=pc: e.tensor_tensor(out=tin[:, 0:1], in0=pr_im, in1=Rim[:, pc:pc + 1], op=ALU.mult), r=rd + [Rim], w=[tin])
                                dve(lambda e, pr_re=pr_re, pc=pc: e.scalar_tensor_tensor(out=ini[:, 0:1], in0=pr_re, scalar=Rre[:, pc:pc + 1], in1=tin[:, 0:1],
                                                                                         op0=ALU.mult, op1=ALU.subtract), r=rd + [Rre, tin], w=[ini])
                                dve(lambda e, pr_im=pr_im, pc=pc: e.tensor_tensor(out=tin[:, 1:2], in0=pr_im, in1=Rre[:, pc:pc + 1], op=ALU.mult), r=rd + [Rre, ini], w=[tin])
                                dve(lambda e, pr_re=pr_re, pc=pc: e.scalar_tensor_tensor(out=ini[:, 1:2], in0=pr_re, scalar=Rim[:, pc:pc + 1], in1=tin[:, 1:2],
                                                                                         op0=ALU.mult, op1=ALU.add), r=rd + [Rim, tin], pw=[ini])
                                dve(lambda e, gsub=gsub: e.tensor_scalar(ini[:], ini[:], keep[:, gsub:gsub + 1], None, ALU.mult), r=[ini, keep], w=[ini])
                                for ri in range(2):
                                    dve(lambda e, ri=ri, sub=sub, zb=zb, wv=wv: e.tensor_tensor_scan(out=wv[:, ri, sub * 128:(sub + 1) * 128], data0=magT[:],
                                                                                                     data1=zb[:, ri, sub * 128:(sub + 1) * 128], initial=ini[:, ri:ri + 1],
                                                                                                     op0=ALU.mult, op1=ALU.add),
                                        r=[magT, zb, ini], w=[wv])
                            dve(lambda e, pc=pc, wv=wv: e.tensor_copy(wend[:, pc, :], wv[:, :, T - 1]), r=[wv], pw=[wend])
                            if own:
                                a4 = pc % 4
                                pool(lambda e, wv=wv, rcb=rcb: e.tensor_tensor(out=r4(ta[:]), in0=r4(wv[:, 0, :]), in1=rcb, op=ALU.mult), r=[wv, rt], w=[ta])
                                pool(lambda e, wv=wv, rsb=rsb: e.tensor_tensor(out=r4(tb[:]), in0=r4(wv[:, 1, :]), in1=rsb, op=ALU.mult), r=[wv, rt], w=[tb])
                                pool(lambda e, a4=a4: e.tensor_tensor(out=sst[:, a4, 0, :], in0=ta[:], in1=tb[:], op=ALU.subtract), r=[ta, tb], pw=[sst])
                                pool(lambda e, wv=wv, rsb=rsb: e.tensor_tensor(out=r4(ta[:]), in0=r4(wv[:, 0, :]), in1=rsb, op=ALU.mult), r=[wv, rt, sst], w=[ta])
                                pool(lambda e, wv=wv, rcb=rcb: e.tensor_tensor(out=r4(tb[:]), in0=r4(wv[:, 1, :]), in1=rcb, op=ALU.mult), r=[wv, rt, sst], w=[tb])
                                pool(lambda e, a4=a4: e.tensor_tensor(out=sst[:, a4, 1, :], in0=ta[:], in1=tb[:], op=ALU.add), r=[ta, tb], pw=[sst])
                                if a4 == 3:
                                    k = 0
                                    for b4 in range(4):
                                        for ri in range(2):
                                            pe(lambda e, b4=b4, ri=ri, uc=uc, k=k: e.matmul(py[:], lhsT=sw[:, 2 + ri, uc * 4 + b4, :], rhs=sst[:, b4, ri, :],
                                                                                           start=(k == 0), stop=(k == 7)), r=[sw, sst], pw=[py])
                                            k += 1
                                    dve(lambda e, uc=uc: e.scalar_tensor_tensor(out=y32[:], in0=u32[:, uc, :], scalar=dsk[:, uc:uc + 1], in1=py[:],
                                                                                op0=ALU.mult, op1=ALU.add), r=[u32, dsk, py], w=[y32])
                                    act(lambda e, uc=uc: e.activation(out=g32[:, uc, :], in_=y32[:], func=AF.Gelu), r=[y32], pw=[g32])
                                    dve(lambda e, uc=uc: e.tensor_copy(gb[:, uc, :], g32[:, uc, :]), r=[g32], pw=[gb])
                        if own:
                            if "yssm0" in dbg and pos == 3:
                                dbgt("yssm0", [128, 4, T]); dump("yssm0", g32[:], [g32])
                            wg = sb(st2, "wg", [128, 4, 4, 128], BF16)
                            sg = sb(st2, "sg", [128, T])
                            for n in range(4):
                                dma(wg.t[:, n], wgl.t[n], r=[wgl], pw=[wg])
                            for n in range(4):
                                for kc in range(4):
                                    pe(lambda e, n=n, kc=kc: e.matmul(py[:], lhsT=wg[:, n, kc, :], rhs=gb[:, kc, :], start=(kc == 0), stop=(kc == 3)),
                                       r=[wg, gb], pw=[py])
                                act(lambda e, n=n: e.activation(out=sg[:], in_=py[:], func=AF.Sigmoid, bias=bglu[:, n:n + 1]), r=[py, bglu], w=[sg])
                                dve(lambda e, n=n: e.tensor_tensor(out=yssmT[:, n, :], in0=g32[:, n, :], in1=sg[:], op=ALU.mult), r=[g32, sg], pw=[yssmT])
                        S.barrier()
                ckpt("ssm%d" % pos)
                QT = sb(tile_st, "QT", [128, H, T], BF16) if own else None
                with contextlib.ExitStack() as st2:
                    wpc = [sb(st2, "wpc%d" % i, [128, 16, 512], BF16) for i in range(2)]
                    kf = sb(st2, "kf", [128, 512]); ksq = sb(st2, "ksq", [128, 512]); kss = sb(st2, "kss", [128, 4])
                    kb_ = sb(st2, "kb_", [128, 512], BF16)
                    KT = sb(st2, "KT", [128, H, T], BF16); Vb = sb(st2, "Vb", [128, NSUB, 1024], BF16)
                    pkv = [ps(st2, "pkv%d" % i, [128, 512]) for i in range(2)]
                    ptb = ps(st2, "ptb", [128, 4, 128], BF16)
                    headnorm_T(st2, hT, (0, 1), kwb, KT, "k", (wpc, pkv, ptb, kf, ksq, kss, kb_))
                    dma(kts.t[:, :, tok0:tok0 + T], KT[:], r=[KT], pw=[kts])
                    if own:
                        headnorm_T(st2, hT, (5, 6), qwb, QT, "q", (wpc, pkv, ptb, kf, ksq, kss, kb_))
                    k = 0
                    for half in range(2):
                        wb = wpc[half % 2]
                        dma(wb.t[:], wp1.t[2 + half], r=[wp1], w=[wb])
                        for sub in range(NSUB):
                            pb = pkv[k % 2]; k += 1
                            for dc in range(16):
                                pe(lambda e, dc=dc, sub=sub, wb=wb, pb=pb: e.matmul(pb[:], lhsT=hT[:, dc, sub * 128:(sub + 1) * 128], rhs=wb[:, dc, :],
                                                                                  start=(dc == 0), stop=(dc == 15)), r=[hT, wb], pw=[pb])
                            act(lambda e, half=half, sub=sub, pb=pb: e.activation(out=Vb[:, sub, half * 512:(half + 1) * 512], in_=pb[:], func=AF.Copy),
                                r=[pb], pw=[Vb])
                    for sub in range(NSUB):
                        dma(vts.t[:, :, pos * NSUB + sub, :], Vb[:, sub, :].rearrange("p (h d) -> p h d", h=H), r=[Vb], pw=[vts])
                    S.barrier()
                ckpt("kv%d" % pos)
                if not own:
                    tile_st.close(); x1_st.close()
                    continue

                yattnT = sb(tile_st, "yattnT", [128, H, T], BF16)
                with contextlib.ExitStack() as st:
                    Kc = [sb(st, "Kc%d" % i, [128, 1024], BF16) for i in range(2)]
                    Vc = [sb(st, "Vc%d" % i, [128, 8, 129], BF16) for i in range(2)]
                    fqb = [sb(st, "fqb%d" % i, [128, T]) for i in range(2)]
                    sS = [sb(st, "sS%d" % i, [128, T]) for i in range(2)]
                    PT = [sb(st, "PT%d" % i, [128, T], BF16) for i in range(2)]
                    yat = sb(st, "yat", [128, NSUB, 1024], BF16)
                    rinv = sb(st, "rinv", [128, 1])
                    pst = [ps(st, "pst%d" % i, [128, T]) for i in range(2)]
                    po = [ps(st, "po%d" % i, [128, 512]) for i in range(NSUB)]
                    pfq = ps(st, "pfq", [128, T])
                    ptr = ps(st, "patr", [128, 4, 128], BF16)
                    for vb in Vc:
                        dve(lambda e, vb=vb: e.memset(vb[:, :, 128:129], 1.0), pw=[vb])
                    nkb = (pos + 1) * NSUB
                    blk = 0; chunk = 0
                    for h in range(H):
                        fq = fqb[h % 2]
                        dve(lambda e, h=h: e.tensor_copy(selh[:], negI[:, h:h + 1].to_broadcast([8, 128])), r=[negI], w=[selh])
                        pe(lambda e, h=h: e.matmul(pfq[:], lhsT=selh[:], rhs=negFT[:], start=True, stop=True), r=[selh, negFT], w=[pfq])
                        act(lambda e, fq=fq: e.activation(out=fq[:], in_=pfq[:], func=AF.Copy), r=[pfq], w=[fq])
                        for kb in range(nkb):
                            if kb % 8 == 0:
                                kc_ = Kc[chunk % 2]; vc_ = Vc[chunk % 2]; chunk += 1
                                n8 = min(8, nkb - kb)
                                dma(kc_.t[:, 0:n8 * 128], kts.t[:, h, kb * 128:(kb + n8) * 128], r=[kts], w=[kc_])
                                dma(vc_.t[:, 0:n8, 0:128], vts.t[:, h, kb:kb + n8, :], r=[vts], pw=[vc_])
                            qlo = max(0, kb - pos * NSUB)
                            c0 = qlo * 128
                            pS = pst[blk % 2]; s_ = sS[blk % 2]; p_ = PT[blk % 2]; blk += 1
                            pe(lambda e, h=h, kb=kb, c0=c0, kc_=kc_, pS=pS: e.matmul(pS[:, c0:T], lhsT=kc_[:, (kb % 8) * 128:(kb % 8 + 1) * 128], rhs=QT[:, h, c0:T],
                                                                                     start=True, stop=True), r=[kc_, QT], w=[pS])
                            dve(lambda e, h=h, kb=kb, c0=c0, pS=pS, s_=s_, fq=fq: e.scalar_tensor_tensor(out=s_[:, c0:T], in0=pS[:, c0:T], scalar=negFk[:, kb, h:h + 1],
                                                                                                       in1=fq[:, c0:T], op0=ALU.add, op1=ALU.add),
                                r=[pS, negFk, fq], w=[s_])
                            if kb >= pos * NSUB:
                                dve(lambda e, c0=c0, s_=s_: e.tensor_tensor(out=s_[:, c0:c0 + 128], in0=s_[:, c0:c0 + 128], in1=maskT[:], op=ALU.add), r=[s_, maskT], w=[s_])
                            act(lambda e, c0=c0, s_=s_, p_=p_: e.activation(out=p_[:, c0:T], in_=s_[:, c0:T], func=AF.Exp), r=[s_], w=[p_])
                            for sub in range(qlo, NSUB):
                                last = pos * NSUB + sub
                                pe(lambda e, sub=sub, kb=kb, p_=p_, vc_=vc_, last=last: e.matmul(po[sub][:, 0:129], lhsT=p_[:, sub * 128:(sub + 1) * 128], rhs=vc_[:, kb % 8, :],
                                                                                              start=(kb == 0), stop=(kb == last)), r=[p_, vc_], pw=[po[sub]])
                        for sub in range(NSUB):
                            dve(lambda e, sub=sub: e.reciprocal(rinv[:], po[sub][:, 128:129]), r=[po[sub]], w=[rinv])
                            dve(lambda e, sub=sub, h=h: e.tensor_scalar(yat[:, sub, h * 128:(h + 1) * 128], po[sub][:, 0:128], rinv[:, 0:1], None, ALU.mult),
                                r=[po[sub], rinv], pw=[yat])
                    for sub in range(NSUB):
                        for half in range(2):
                            for a in range(4):
                                h = half * 4 + a
                                pe(lambda e, a=a, h=h, sub=sub: e.transpose(out=ptr[:, a, :], in_=yat[:, sub, h * 128:(h + 1) * 128], identity=identb[:]),
                                   r=[yat, identb], pw=[ptr])
                            act(lambda e, half=half, sub=sub: e.activation(out=yattnT[:, half * 4:(half + 1) * 4, sub * 128:(sub + 1) * 128], in_=ptr[:], func=AF.Copy),
                                r=[ptr], pw=[yattnT])
                    if "yattn" in dbg and pos == 3:
                        dbgt("yattn", [128, H, T])
                        ydb = sb(st, "ydb", [128, H, T])
                        dve(lambda e: e.tensor_copy(ydb[:], yattnT[:]), r=[yattnT], w=[ydb])
                        dump("yattn", ydb[:], [ydb])
                    S.barrier()

                ckpt("attn%d" % pos)
                def add_back(st, srcT, gx, nm):
                    dT = [sb(st, nm + "dT%d" % i, [128, T]) for i in range(2)]
                    pt_ = [ps(st, nm + "pt%d" % i, [128, NSUB, 128]) for i in range(2)]
                    for n in range(16):
                        d_ = dT[n % 2]; pb = pt_[n % 2]
                        sap, stt = srcT(n)
                        act(lambda e, n=n, d_=d_, sap=sap: e.activation(out=d_[:], in_=sap, func=AF.Copy, scale=gx[:, n:n + 1]), r=[stt, gx], w=[d_])
                        for sub in range(NSUB):
                            pe(lambda e, sub=sub, d_=d_, pb=pb: e.transpose(out=pb[:, sub, :], in_=d_[:, sub * 128:(sub + 1) * 128], identity=identf[:]),
                               r=[d_, identf], pw=[pb])
                        dve(lambda e, n=n, pb=pb: e.tensor_tensor(out=x1[:, :, n * 128:(n + 1) * 128], in0=x1[:, :, n * 128:(n + 1) * 128], in1=pb[:], op=ALU.add),
                            r=[pb], pw=[x1])

                with contextlib.ExitStack() as st:
                    merged = sb(st, "merged", [128, 16, T], BF16)
                    wmb = [sb(st, "wmb%d" % i, [128, 44, 128], BF16) for i in range(2)]
                    sgs = sb(st, "sgs", [128, T]); sga = sb(st, "sga", [128, T]); m1 = sb(st, "m1", [128, T]); m2 = sb(st, "m2", [128, T])
                    pmg = [ps(st, "pmg%d" % i, [128, T]) for i in range(4)]
                    for n in range(16):
                        wb = wmb[n % 2]
                        dma(wb.t[:], wm.t[n], r=[wm], w=[wb])
                        for kc in range(4):
                            pe(lambda e, kc=kc, wb=wb: e.matmul(pmg[0][:], lhsT=wb[:, kc, :], rhs=yssmT[:, kc, :], start=(kc == 0), stop=(kc == 3)), r=[wb, yssmT], pw=[pmg[0]])
                        for kc in range(8):
                            pe(lambda e, kc=kc, wb=wb: e.matmul(pmg[1][:], lhsT=wb[:, 4 + kc, :], rhs=yattnT[:, kc, :], start=(kc == 0), stop=(kc == 7)), r=[wb, yattnT], pw=[pmg[1]])
                        for kc in range(16):
                            pe(lambda e, kc=kc, wb=wb: e.matmul(pmg[2][:], lhsT=wb[:, 12 + kc, :], rhs=hT[:, kc, :], start=(kc == 0), stop=(kc == 15)), r=[wb, hT], pw=[pmg[2]])
                        for kc in range(16):
                            pe(lambda e, kc=kc, wb=wb: e.matmul(pmg[3][:], lhsT=wb[:, 28 + kc, :], rhs=hT[:, kc, :], start=(kc == 0), stop=(kc == 15)), r=[wb, hT], pw=[pmg[3]])
                        act(lambda e: e.activation(out=sgs[:], in_=pmg[2][:], func=AF.Sigmoid), r=[pmg[2]], w=[sgs])
                        act(lambda e: e.activation(out=sga[:], in_=pmg[3][:], func=AF.Sigmoid), r=[pmg[3]], w=[sga])
                        dve(lambda e: e.tensor_tensor(out=m1[:], in0=sgs[:], in1=pmg[0][:], op=ALU.mult), r=[sgs, pmg[0]], w=[m1])
                        dve(lambda e: e.tensor_tensor(out=m2[:], in0=sga[:], in1=pmg[1][:], op=ALU.mult), r=[sga, pmg[1]], w=[m2])
                        pool(lambda e, n=n: e.tensor_tensor(out=merged[:, n, :], in0=m1[:], in1=m2[:], op=ALU.add), r=[m1, m2], pw=[merged])
                    S.barrier()
                    with contextlib.ExitStack() as st2:
                        wob = [sb(st2, "wob%d" % i, [128, 16, 128], BF16) for i in range(2)]
                        pop = [ps(st2, "pop%d" % i, [128, T]) for i in range(2)]

                        def srcT(n):
                            wb = wob[n % 2]; pb = pop[n % 2]
                            dma(wb.t[:], wo.t[n], r=[wo], w=[wb])
                            for kc in range(16):
                                pe(lambda e, kc=kc, wb=wb, pb=pb: e.matmul(pb[:], lhsT=wb[:, kc, :], rhs=merged[:, kc, :], start=(kc == 0), stop=(kc == 15)),
                                   r=[wb, merged], pw=[pb])
                            return pb[:], pb
                        add_back(st2, srcT, g1, "o")
                        S.barrier()
                    S.barrier()
                if "x1" in dbg and pos == 3:
                    dbgt("x1", [128, NSUB, D]); dump("x1", x1[:], [x1])

                ckpt("merge%d" % pos)
                S.barrier()
                tile_st.close()
                with contextlib.ExitStack() as st:
                    h2T = sb(st, "h2T", [128, 16, T], BF16)
                    with contextlib.ExitStack() as st2:
                        rmsnorm_to_fm(st2, lambda sub: (x1[:, sub, :], x1), sc2, bi2, h2T, "n2")
                        S.barrier()
                    scr_ = sb(st, "scr_", [128, NSUB, 16, 128])
                    thr = sb(st, "thr", [128, NSUB, H]); negc = sb(st, "negc", [128, NSUB, H])
                    with contextlib.ExitStack() as st2:
                        qT = sb(st2, "qT", [128, 16, T], BF16)
                        wqb = [sb(st2, "wqb%d" % i, [128, 16, 128], BF16) for i in range(2)]
                        pq = [ps(st2, "pq%d" % i, [128, T]) for i in range(2)]
                        psc = [ps(st2, "psc%d" % i, [128, 4, 128]) for i in range(2)]
                        for c in range(16):
                            wb = wqb[c % 2]; pb = pq[c % 2]
                            dma(wb.t[:], wqs.t[c], r=[wqs], w=[wb])
                            for kc in range(16):
                                pe(lambda e, kc=kc, wb=wb, pb=pb: e.matmul(pb[:], lhsT=wb[:, kc, :], rhs=h2T[:, kc, :], start=(kc == 0), stop=(kc == 15)), r=[wb, h2T], pw=[pb])
                            act(lambda e, c=c, pb=pb: e.activation(out=qT[:, c, :], in_=pb[:], func=AF.Copy), r=[pb], pw=[qT])
                        k = 0
                        for sub in range(NSUB):
                            for q4 in range(4):
                                pb = psc[k % 2]; k += 1
                                for a in range(4):
                                    c = q4 * 4 + a
                                    pe(lambda e, a=a, c=c, sub=sub, pb=pb: e.matmul(pb[:, a, :], lhsT=qT[:, c, sub * 128:(sub + 1) * 128], rhs=keysb[:, c, :], start=True, stop=True),
                                       r=[qT, keysb], pw=[pb])
                                act(lambda e, sub=sub, q4=q4, pb=pb: e.activation(out=scr_[:, sub, q4 * 4:(q4 + 1) * 4, :], in_=pb[:], func=AF.Copy), r=[pb], pw=[scr_])
                        v16 = sb(st2, "v16", [128, 2, 16]); tmp128 = sb(st2, "tmp128", [128, 128])
                        cand = sb(st2, "cand", [128, 256]); cand2 = sb(st2, "cand2", [128, 256]); tv = sb(st2, "tv", [128, 16])
                        ez = sb(st2, "ez", [128, 16]); zs = sb(st2, "zs", [128, 1]); nm_ = sb(st2, "nm_", [128, 1])
                        for sub in range(NSUB):
                            for h in range(H):
                                for p in range(2):
                                    sv = scr_[:, sub, h * 2 + p, :]
                                    dve(lambda e, p=p, sv=sv: e.max(out=v16[:, p, 0:8], in_=sv), r=[scr_], pw=[v16])
                                    dve(lambda e, p=p, sv=sv: e.match_replace(out=tmp128[:], in_to_replace=v16[:, p, 0:8], in_values=sv, imm_value=-1e30), r=[scr_, v16], w=[tmp128])
                                    dve(lambda e, p=p: e.max(out=v16[:, p, 8:16], in_=tmp128[:]), r=[tmp128], pw=[v16])
                                dve(lambda e: e.tensor_tensor(out=cand[:].rearrange("p (a b) -> p a b", a=16), in0=v16[:, 0, :].unsqueeze(2).to_broadcast([128, 16, 16]),
                                                              in1=v16[:, 1, :].unsqueeze(1).to_broadcast([128, 16, 16]), op=ALU.add), r=[v16], w=[cand])
                                dve(lambda e: e.max(out=tv[:, 0:8], in_=cand[:]), r=[cand], pw=[tv])
                                dve(lambda e: e.match_replace(out=cand2[:], in_to_replace=tv[:, 0:8], in_values=cand[:], imm_value=-1e30), r=[cand, tv], w=[cand2])
                                dve(lambda e: e.max(out=tv[:, 8:16], in_=cand2[:]), r=[cand2], pw=[tv])
                                dve(lambda e, sub=sub, h=h: e.tensor_copy(thr[:, sub, h:h + 1], tv[:, 15:16]), r=[tv], pw=[thr])
                                dve(lambda e: e.tensor_scalar(nm_[:], tv[:, 0:1], -1.0, None, ALU.mult), r=[tv], w=[nm_])
                                act(lambda e: e.activation(out=ez[:], in_=tv[:], func=AF.Exp, bias=nm_[:, 0:1], accum_out=zs[:, 0:1]), r=[tv, nm_], w=[ez, zs])
                                act(lambda e: e.activation(out=zs[:], in_=zs[:], func=AF.Ln), r=[zs], w=[zs])
                                dve(lambda e, sub=sub, h=h: e.tensor_tensor(out=negc[:, sub, h:h + 1], in0=nm_[:], in1=zs[:], op=ALU.subtract), r=[nm_, zs], pw=[negc])
                        S.barrier()
                    acc = sb(st, "acc", [128, 16, T])
                    WTs = [sb(st, "WT%d" % i, [128, GE, T], BF16) for i in range(2)]
                    Sgs = [sb(st, "Sg%d" % i, [128, GE, 128]) for i in range(2)]
                    Egs = [sb(st, "Eg%d" % i, [128, GE, 128], BF16) for i in range(2)]
                    Gts = [sb(st, "Gt%d" % i, [128, GE, 128], BF16) for i in range(2)]
                    Gas = [sb(st, "Ga%d" % i, [128, GE, 128], BF16) for i in range(2)]
                    kgat = 0
                    ub = [sb(st, "ub%d" % i, [128, 16, 128], BF16) for i in range(3)]
                    vb_ = [sb(st, "vb%d" % i, [128, GE, 128], BF16) for i in range(2)]
                    pa = [ps(st, "pa%d" % i, [128, T]) for i in range(2)]
                    pg = [ps(st, "pg%d" % i, [128, 4, 128], BF16) for i in range(2)]
                    pv = [ps(st, "pv%d" % i, [128, T]) for i in range(2)]
                    pusv = pus.t.rearrange("(i p) (c e) -> i p c e", p=128, c=16)
                    pvsv = pvs.t.rearrange("(g c p) (i d) -> g c p i d", c=16, p=128, i=GE)
                    ku = 0; kv = 0; kg = 0
                    def emit_AT(g, ku):
                        WTg = WTs[g % 2]
                        for il in range(GE):
                            i = g * GE + il
                            u_ = ub[ku % 3]; pb = pa[ku % 2]; ku += 1
                            dma(u_.t[:], pusv[i], r=[pus], w=[u_])
                            for dc in range(16):
                                pe(lambda e, dc=dc, u_=u_, pb=pb: e.matmul(pb[:], lhsT=u_[:, dc, :], rhs=h2T[:, dc, :], start=(dc == 0), stop=(dc == 15)), r=[u_, h2T], pw=[pb])
                            act(lambda e, il=il, pb=pb, WTg=WTg: e.activation(out=WTg[:, il, :], in_=pb[:], func=AF.Gelu), r=[pb], pw=[WTg])
                        return ku

                    ku = emit_AT(0, ku)
                    for g in range(NG):
                        WT = WTs[g % 2]
                        if g + 1 < NG:
                            ku = emit_AT(g + 1, ku)
                        for sub in range(NSUB):
                            Ga = Gas[(g * NSUB + sub) % 2]
                            for h in range(H):
                                Sg = Sgs[kgat % 2]; Eg = Egs[kgat % 2]; Gt = Gts[kgat % 2]; kgat += 1
                                s1 = scr_[:, sub, h * 2, g * GE:(g + 1) * GE].unsqueeze(2).to_broadcast([128, GE, 128])
                                s2 = scr_[:, sub, h * 2 + 1, :].unsqueeze(1).to_broadcast([128, GE, 128])
                                dve(lambda e, s1=s1, s2=s2: e.tensor_tensor(out=Sg[:], in0=s1, in1=s2, op=ALU.add), r=[scr_], w=[Sg])
                                act(lambda e, sub=sub, h=h: e.activation(out=Eg[:], in_=Sg[:], func=AF.Exp, bias=negc[:, sub, h:h + 1]), r=[Sg, negc], w=[Eg])
                                gdst = Ga if h == 0 else Gt
                                dve(lambda e, sub=sub, h=h, gdst=gdst: e.scalar_tensor_tensor(out=gdst[:].rearrange("p a b -> p (a b)"), in0=Sg[:].rearrange("p a b -> p (a b)"),
                                                                                              scalar=thr[:, sub, h:h + 1], in1=Eg[:].rearrange("p a b -> p (a b)"),
                                                                                              op0=ALU.is_ge, op1=ALU.mult), r=[Sg, Eg, thr], w=[gdst])
                                if h > 0:
                                    pool(lambda e: e.tensor_tensor(out=Ga[:], in0=Ga[:], in1=Gt[:], op=ALU.add), r=[Ga, Gt], w=[Ga])
                            for q4 in range(GE // 4):
                                pb = pg[kg % 2]; kg += 1
                                for a in range(4):
                                    pe(lambda e, a=a, q4=q4, pb=pb: e.transpose(out=pb[:, a, :], in_=Ga[:, q4 * 4 + a, :], identity=identb[:]), r=[Ga, identb], pw=[pb])
                                dve(lambda e, q4=q4, sub=sub, pb=pb: e.tensor_tensor(out=WT[:, q4 * 4:(q4 + 1) * 4, sub * 128:(sub + 1) * 128],
                                                                                    in0=WT[:, q4 * 4:(q4 + 1) * 4, sub * 128:(sub + 1) * 128], in1=pb[:], op=ALU.mult),
                                    r=[pb, WT], pw=[WT])
                        for dc in range(16):
                            v_ = vb_[kv % 2]; pb = pv[kv % 2]; kv += 1
                            dma(v_.t[:], pvsv[g, dc], r=[pvs], w=[v_])
                            for il in range(GE):
                                pe(lambda e, il=il, v_=v_, pb=pb: e.matmul(pb[:], lhsT=v_[:, il, :], rhs=WT[:, il, :], start=(il == 0), stop=(il == GE - 1)), r=[v_, WT], pw=[pb])
                            if g == 0:
                                act(lambda e, dc=dc, pb=pb: e.activation(out=acc[:, dc, :], in_=pb[:], func=AF.Copy), r=[pb], pw=[acc])
                            else:
                                dve(lambda e, dc=dc, pb=pb: e.tensor_tensor(out=acc[:, dc, :], in0=acc[:, dc, :], in1=pb[:], op=ALU.add), r=[pb, acc], pw=[acc])
                    S.barrier()
                    with contextlib.ExitStack() as st2:
                        add_back(st2, lambda n: (acc[:, n, :], acc), g2, "p")
                        S.barrier()
                    for sub in range(NSUB):
                        dma(out_t[oi * T + sub * 128:oi * T + (sub + 1) * 128, :], x1[:, sub, :], r=[x1])
                    S.barrier()
                S.barrier()
                x1_st.close()
        S.barrier()
        print("bass instructions:", S.ninst, "sems:", S.nsem)
    return nc, list(dbg_out)


def _prep_inputs(inp):
    f = np.float32
    g = lambda k: np.asarray(inp[k], dtype=f)
    pm = lambda v, n: np.ascontiguousarray(v.reshape(n, 128).T)
    common = {}
    common["w_ada"] = np.ascontiguousarray(g("w_ada")[0])
    common["b_ada"] = pm(g("b_ada")[0], 96)
    common["norm1_w"] = pm(g("norm1_w")[0], 16)
    common["norm2_w"] = pm(g("norm2_w")[0], 16)
    common["w_in"] = np.ascontiguousarray(g("w_in")[0])
    common["negbf"] = np.ascontiguousarray(-g("b_forget")[0].reshape(8, 1)) if False else None
    common["qw_bc"] = np.ascontiguousarray(np.tile(g("q_norm_w")[0][None, :], (128, 4)))
    common["kw_bc"] = np.ascontiguousarray(np.tile(g("k_norm_w")[0][None, :], (128, 4)))
    common["a_re"] = pm(g("ssm_A_re")[0].reshape(-1), 16)
    common["a_im"] = pm(g("ssm_A_im")[0].reshape(-1), 16)
    common["log_dt"] = pm(np.repeat(g("ssm_log_dt")[0], 64), 16)

    def placeB(B):
        o = np.zeros((128, 16, 128), f)
        for gi in range(32):
            o[(gi % 2) * 64:(gi % 2) * 64 + 64, gi // 2, (gi % 8) * 16:(gi % 8) * 16 + 16] = B[gi]
        return o
    common["b_re_pl"] = placeB(g("ssm_B_re")[0]); common["b_im_pl"] = placeB(g("ssm_B_im")[0])
    common["c_re_pl"] = placeB(g("ssm_C_re")[0].transpose(0, 2, 1)); common["c_im_pl"] = placeB(g("ssm_C_im")[0].transpose(0, 2, 1))
    common["d_skip"] = pm(g("ssm_D")[0].reshape(-1), 4)
    common["w_glu"] = np.ascontiguousarray(g("w_glu")[0]); common["b_glu"] = pm(g("b_glu")[0], 4)
    common["w_ssm_up"] = np.ascontiguousarray(g("w_ssm_up")[0]); common["w_attn_up"] = np.ascontiguousarray(g("w_attn_up")[0])
    common["w_out"] = np.ascontiguousarray(g("w_out")[0]); common["w_peer_q"] = np.ascontiguousarray(g("w_peer_q")[0])
    common["keysT"] = np.ascontiguousarray(g("peer_sub_keys")[0].reshape(16, 128, 128).transpose(2, 0, 1))
    pu = g("peer_u")[0].reshape(128, 128, 16, 128)
    common["peer_uT"] = np.ascontiguousarray(pu.transpose(0, 3, 2, 1)).reshape(128 * 128, 2048)
    pv = g("peer_v")[0].reshape(NG, GE, 128, 16, 128)
    common["peer_vL"] = np.ascontiguousarray(pv.transpose(0, 3, 2, 1, 4)).reshape(NG * 16 * 128, GE * 128)
    common["ident"] = np.eye(128, dtype=f)
    kk = np.arange(128)
    common["maskT"] = np.where(kk[:, None] <= kk[None, :], 0.0, -30000.0).astype(f)
    del common["negbf"]
    common["negbf"] = np.ascontiguousarray(g("b_forget")[0].reshape(8, 1))
    maps = []
    x = g("x"); c = g("c")
    for core in range(8):
        b, j = core // 4, core % 4
        pad = (3 - j) * T
        m = dict(common)
        xs = np.zeros((NPOS * T, D), f)
        xs[pad:] = x[b, :NPOS * T - pad]
        m["x"] = xs
        m["c"] = pm(c[b], 16)
        fp = np.zeros((8, NPOS * T), f)
        kp = np.ones((128, NPOS * NSUB), f)
        if pad > 0:
            fp[:, pad] = PADF
            kp[:, pad // 128] = 0.0
        m["fpad"] = fp; m["keep"] = kp
        maps.append(m)
    return maps


def kernel(**inputs):
    maps = _prep_inputs(inputs)
    nc, _ = build()
    res = run_bass_kernel_spmd(nc, maps, core_ids=list(range(8)))
    out = np.zeros((2, SEQ, D), np.float32)
    for core in range(8):
        b, j = core // 4, core % 4
        o = np.asarray(res.results[core]["out"], dtype=np.float32).reshape(NPOS // 4, T, D)
        for m in range(NPOS // 4):
            t = 4 * m + j
            out[b, t * T:(t + 1) * T] = o[m]
    return out
```

```python
import contextlib
import numpy as np
import concourse.bass as bass
import concourse.mybir as mybir
from concourse.bass_utils import run_bass_kernel_spmd

F32 = mybir.dt.float32
BF16 = mybir.dt.bfloat16
ALU = mybir.AluOpType
AF = mybir.ActivationFunctionType
AX = mybir.AxisListType

D = 2048
SEQ = 16384
T = 512
NPOS = 32
NSUB = 4
H = 8
GE = 8
NG = 128 // GE
EPS = 1e-6
PADF = 300.0


class Buf:
    __slots__ = ("wf", "wp", "r")

    def __init__(self):
        self.wf = {}; self.wp = {}; self.r = {}


class Sched:
    SAME = True
    ROT = 30000
    NDMA = 16

    def __init__(self, nc, es):
        self.nc = nc; self.es = es
        self.eng = {"pe": nc.tensor, "act": nc.scalar, "dve": nc.vector, "pool": nc.gpsimd, "sp": nc.sync}
        self.nsem = 0
        self.sem = {}; self.cnt = {}; self.waited = {e: {} for e in self.eng}
        for e in self.eng:
            self.sem[e] = self._newsem(); self.cnt[e] = 0
        self.dsem = [self._newsem() for _ in range(self.NDMA)]
        self.dcnt = [0] * self.NDMA
        self.dnext = 0; self.pnext = 0
        self.alltok = {}
        self.ninst = 0

    def _newsem(self):
        self.nsem += 1
        return self.es.enter_context(self.nc.semaphore("sm%d" % self.nsem))

    def _wait(self, e, toks):
        w = self.waited[e]
        best = {}
        for (s, v) in toks:
            if (not self.SAME) and s is self.sem[e]:
                continue
            k = id(s)
            if w.get(k, 0) >= v:
                continue
            if k not in best or best[k][1] < v:
                best[k] = (s, v)
        for k, (s, v) in best.items():
            self.eng[e].wait_ge(s, v)
            w[k] = v

    @staticmethod
    def _put(d, tok):
        k = id(tok[0])
        if k not in d or d[k][1] < tok[1]:
            d[k] = tok

    def _deps(self, reads, writes, pwrites):
        toks = []
        for b in reads:
            toks.extend(b.wf.values()); toks.extend(b.wp.values())
        for b in writes:
            toks.extend(b.wf.values()); toks.extend(b.wp.values()); toks.extend(b.r.values())
        for b in pwrites:
            toks.extend(b.wf.values()); toks.extend(b.r.values())
        return toks

    def _commit(self, tok, reads, writes, pwrites):
        for b in writes:
            b.wf = {id(tok[0]): tok}; b.wp = {}; b.r = {}
        for b in pwrites:
            if b.r:
                b.wf = {}; b.wp = {}; b.r = {}
            self._put(b.wp, tok)
        for b in reads:
            self._put(b.r, tok)
        self.alltok[id(tok[0])] = tok

    dead = False

    def op(self, e, fn, reads=(), writes=(), pwrites=()):
        if self.dead:
            return None
        self._wait(e, self._deps(reads, writes, pwrites))
        if self.cnt[e] >= self.ROT:
            self.sem[e] = self._newsem(); self.cnt[e] = 0
        inst = fn(self.eng[e])
        self.cnt[e] += 1; self.ninst += 1
        inst.then_inc(self.sem[e], 1)
        tok = (self.sem[e], self.cnt[e])
        self._commit(tok, reads, writes, pwrites)
        return tok

    def dma(self, q, out, in_, reads=(), writes=(), pwrites=()):
        if q == "pool":
            i = self.NDMA - 2 + self.pnext; self.pnext = (self.pnext + 1) % 2
        else:
            i = self.dnext; self.dnext = (self.dnext + 1) % (self.NDMA - 2)
        if self.dead:
            return None
        toks = self._deps(reads, writes, pwrites)
        if self.dcnt[i] > 0:
            toks.append((self.dsem[i], 16 * self.dcnt[i]))
        self._wait(q, toks)
        inst = self.eng[q].dma_start(out=out, in_=in_)
        self.dcnt[i] += 1; self.ninst += 1
        inst.then_inc(self.dsem[i], 16)
        tok = (self.dsem[i], 16 * self.dcnt[i])
        self._commit(tok, reads, writes, pwrites)
        return tok

    def barrier(self):
        if self.dead:
            return
        toks = list(self.alltok.values())
        for e in self.eng:
            self._wait(e, toks)


class TT:
    def __init__(self, t):
        self.t = t; self.b = Buf()

    def __getitem__(self, k):
        return self.t[k]


class _Stop(Exception):
    pass


EVAC = [1]


def build(npos=NPOS, dbg=(), stop=None, fast=False):
    nc = bass.Bass("TRN2", target_bir_lowering=False)
    dbg = set(dbg)
    nown = npos // 4

    def din(name, shape, dt=F32):
        return nc.dram_tensor(name, list(shape), dt, kind="ExternalInput").ap()

    x_in = din("x", [NPOS * T, D])
    c_in = din("c", [128, 16])
    wada_in = din("w_ada", [D, 6 * D])
    bada_in = din("b_ada", [128, 96])
    n1_in = din("norm1_w", [128, 16])
    n2_in = din("norm2_w", [128, 16])
    win_in = din("w_in", [D, 7688])
    negbf_in = din("negbf", [8, 1])
    qw_in = din("qw_bc", [128, 512])
    kw_in = din("kw_bc", [128, 512])
    are_in = din("a_re", [128, 16]); aim_in = din("a_im", [128, 16]); ldt_in = din("log_dt", [128, 16])
    bre_in = din("b_re_pl", [128, 16, 128]); bim_in = din("b_im_pl", [128, 16, 128])
    cre_in = din("c_re_pl", [128, 16, 128]); cim_in = din("c_im_pl", [128, 16, 128])
    dsk_in = din("d_skip", [128, 4])
    wglu_in = din("w_glu", [512, 512]); bglu_in = din("b_glu", [128, 4])
    wsu_in = din("w_ssm_up", [512, D]); wau_in = din("w_attn_up", [1024, D]); wout_in = din("w_out", [D, D])
    wq_in = din("w_peer_q", [D, D])
    keys_in = din("keysT", [128, 16, 128])
    pu_in = din("peer_uT", [128 * 128, 2048])
    pv_in = din("peer_vL", [NG * 16 * 128, GE * 128])
    ident_in = din("ident", [128, 128])
    mask_in = din("maskT", [128, 128])
    fpad_in = din("fpad", [8, NPOS * T])
    keep_in = din("keep", [128, NPOS * NSUB])
    out_t = nc.dram_tensor("out", [NPOS // 4 * T, D], F32, kind="ExternalOutput").ap()
    dbg_out = {}
    def dbgt(name, shape):
        if name in dbg:
            dbg_out[name] = nc.dram_tensor("dbg_" + name, list(shape), F32, kind="ExternalOutput").ap()

    def dscr(name, shape, dt=BF16):
        return TT(nc.dram_tensor(name, list(shape), dt).ap())
    wp1 = dscr("wp1", [7, 128, 16, 512])
    wm = dscr("wm", [16, 128, 44, 128])
    wo = dscr("wo", [16, 128, 16, 128])
    wqs = dscr("wqs", [16, 128, 16, 128])
    wgl = dscr("wgl", [4, 128, 4, 128])
    pus = dscr("pus", [128 * 128, 2048])
    pvs = dscr("pvs", [NG * 16 * 128, GE * 128])
    kts = dscr("kts", [128, H, NPOS * T])
    vts = dscr("vts", [128, H, NPOS * NSUB, 128])
    ssmw = dscr("ssmw", [128, 4, 16, 128])
    ssmt = dscr("ssmt", [128, 2, 16, 128], F32)

    with contextlib.ExitStack() as es:
        S = Sched(nc, es)

        uid = [0]

        def sb(st, name, shape, dt=F32):
            uid[0] += 1
            return TT(st.enter_context(nc.sbuf_tensor("%s_%d" % (name, uid[0]), list(shape), dt)))

        def ps(st, name, shape, dt=F32):
            uid[0] += 1
            return TT(st.enter_context(nc.psum_tensor("%s_%d" % (name, uid[0]), list(shape), dt)))

        def dve(fn, r=(), w=(), pw=()):
            return S.op("dve", fn, [t.b for t in r], [t.b for t in w], [t.b for t in pw])

        def act(fn, r=(), w=(), pw=()):
            return S.op("act", fn, [t.b for t in r], [t.b for t in w], [t.b for t in pw])

        def pool(fn, r=(), w=(), pw=()):
            return S.op("pool", fn, [t.b for t in r], [t.b for t in w], [t.b for t in pw])

        pool = dve

        def pe(fn, r=(), w=(), pw=()):
            return S.op("pe", fn, [t.b for t in r], [t.b for t in w], [t.b for t in pw])

        def dma(out, in_, r=(), w=(), pw=(), q="sp"):
            return S.dma(q, out, in_, [t.b for t in r], [t.b for t in w], [t.b for t in pw])

        def dump(name, ap, r):
            if name in dbg_out:
                dma(dbg_out[name], ap, r=r)

        def ckpt(name):
            if stop == name:
                S.barrier()
                S.dead = True

        identf = sb(es, "identf", [128, 128]); identb = sb(es, "identb", [128, 128], BF16)
        maskT = sb(es, "maskT", [128, 128])
        selh = sb(es, "selh", [8, 128]); negI = sb(es, "negI", [8, 8])
        sc1 = sb(es, "sc1", [128, 16]); bi1 = sb(es, "bi1", [128, 16]); g1 = sb(es, "g1", [128, 16])
        sc2 = sb(es, "sc2", [128, 16]); bi2 = sb(es, "bi2", [128, 16]); g2 = sb(es, "g2", [128, 16])
        qwb = sb(es, "qwb", [128, 512]); kwb = sb(es, "kwb", [128, 512])
        wfb = sb(es, "wfb", [128, 16, 8], BF16)
        negbf = sb(es, "negbf", [8, 1])
        mag = sb(es, "mag", [128, 16]); Rre = sb(es, "Rre", [128, 16]); Rim = sb(es, "Rim", [128, 16])
        wend = sb(es, "wend", [128, 16, 2])
        dsk = sb(es, "dsk", [128, 4]); bglu = sb(es, "bglu", [128, 4])
        negFk = sb(es, "negFk", [128, NPOS * NSUB, 8])
        negFT = sb(es, "negFT", [8, T]); fcar = sb(es, "fcar", [8, 1])
        keep = sb(es, "keep", [128, NPOS * NSUB])
        onesT = sb(es, "onesT", [128, T])

        cdma = lambda dst, src: dma(dst.t[:], src, w=[dst])
        cdma(identf, ident_in[:, :]); cdma(maskT, mask_in[:, :])
        cdma(qwb, qw_in[:, :]); cdma(kwb, kw_in[:, :]); cdma(negbf, negbf_in[:, :])
        cdma(dsk, dsk_in[:, :]); cdma(bglu, bglu_in[:, :]); cdma(keep, keep_in[:, :])
        dve(lambda e: e.tensor_copy(identb[:], identf[:]), r=[identf], w=[identb])
        dve(lambda e: e.tensor_scalar(negI[:], identf[0:8, 0:8], -1.0, None, ALU.mult), r=[identf], w=[negI])
        dve(lambda e: e.tensor_scalar(negbf[:], negbf[:], -1.0, None, ALU.mult), r=[negbf], w=[negbf])
        dve(lambda e: e.tensor_scalar(qwb[:], qwb[:], float(128 ** -0.5), None, ALU.mult), r=[qwb], w=[qwb])
        dve(lambda e: e.memset(onesT[:], 1.0), w=[onesT])
        dve(lambda e: e.memset(wend[:], 0.0), w=[wend])
        dve(lambda e: e.memset(fcar[:], 0.0), w=[fcar])

        winv = win_in.rearrange("(c p) n -> p c n", p=128)
        wsuv = wsu_in.rearrange("(c p) n -> p c n", p=128)
        wauv = wau_in.rearrange("(c p) n -> p c n", p=128)
        woutv = wout_in.rearrange("(c p) n -> p c n", p=128)
        wqv = wq_in.rearrange("(c p) n -> p c n", p=128)
        wgluv = wglu_in.rearrange("(c p) n -> p c n", p=128)
        keysb = sb(es, "keysb", [128, 16, 128], BF16)
        cvk = [0]

        def convert_all(parts):
            with contextlib.ExitStack() as st:
                stg = [sb(st, "stg%d" % i, [128, 8192]) for i in range(2)]
                stb = [sb(st, "stb%d" % i, [128, 8192], BF16) for i in range(2)]
                for (loads, n_el, stores) in parts:
                    k = cvk[0]; cvk[0] += 1
                    s32 = stg[k % 2]; s16 = stb[k % 2]
                    for (off, shp, src_ap) in loads:
                        dst = s32[:, off:off + int(np.prod(shp))]
                        if len(shp) == 2:
                            dst = dst.rearrange("p (a b) -> p a b", a=shp[0])
                        dma(dst, src_ap, pw=[s32])
                    eng = (act, dve, pool)[k % 3]
                    if eng is act:
                        act(lambda e: e.activation(out=s16[:, 0:n_el], in_=s32[:, 0:n_el], func=AF.Copy), r=[s32], w=[s16])
                    else:
                        eng(lambda e: e.tensor_copy(s16[:, 0:n_el], s32[:, 0:n_el]), r=[s32], w=[s16])
                    for (off, shp, dst_ap, dtt, *insb) in stores:
                        s_ = s16[:, off:off + int(np.prod(shp))]
                        if len(shp) == 2:
                            s_ = s_.rearrange("p (a b) -> p a b", a=shp[0])
                        if insb:
                            dve(lambda e: e.tensor_copy(dst_ap, s_), r=[s16], w=[dtt])
                        else:
                            dma(dst_ap, s_, r=[s16], pw=[dtt])
                S.barrier()

        def parts_first():
            parts = []
            pcols = [1024, 1536, 2048, 2560, 3080, 0, 512]
            for i, c0 in enumerate(pcols):
                parts.append(([(0, (16, 512), winv[:, :, c0:c0 + 512])], 8192, [(0, (16, 512), wp1.t[i], wp1)]))
            parts.append(([(0, (16, 8), winv[:, :, 3072:3080])], 128, [(0, (16, 8), wfb[:], wfb, True)]))
            parts.append(([(0, (16, 128), keys_in[:, :, :])], 2048, [(0, (16, 128), keysb[:], keysb, True)]))
            return parts

        def parts_rest():
            parts = []
            for n in range(4):
                parts.append(([(0, (4, 128), wgluv[:, :, n * 128:(n + 1) * 128])], 512, [(0, (4, 128), wgl.t[n], wgl)]))
            for n in range(16):
                ns = slice(n * 128, (n + 1) * 128)
                parts.append(([(0, (4, 128), wsuv[:, :, ns]), (512, (8, 128), wauv[:, :, ns]),
                               (1536, (16, 128), winv[:, :, 3592 + n * 128:3592 + (n + 1) * 128]),
                               (3584, (16, 128), winv[:, :, 5640 + n * 128:5640 + (n + 1) * 128])], 5632,
                              [(0, (44, 128), wm.t[n], wm)]))
                parts.append(([(0, (16, 128), woutv[:, :, ns]), (2048, (16, 128), wqv[:, :, ns])], 4096,
                              [(0, (16, 128), wo.t[n], wo), (2048, (16, 128), wqs.t[n], wqs)]))
            for (src_, dst_, nrows, ncols) in ((pu_in, pus, 128 * 128, 2048), (pv_in, pvs, NG * 16 * 128, GE * 128)):
                rr = 8192 // ncols
                for k in range(nrows // (128 * rr)):
                    rs_ = slice(k * 128 * rr, (k + 1) * 128 * rr)
                    parts.append(([(0, (8192,), src_[rs_, :].rearrange("(p r) c -> p (r c)", r=rr))], 8192,
                                  [(0, (8192,), dst_.t[rs_, :].rearrange("(p r) c -> p (r c)", r=rr), dst_)]))
            return parts

        if not fast:
            convert_all(parts_first())

        def conv_rest():
            convert_all(parts_rest())

        ckpt("consts")
        if fast:
            for t_ in (sc1, bi1, g1, sc2, bi2, g2):
                dve(lambda e, t_=t_: e.memset(t_[:], 0.5), w=[t_])
        with contextlib.ExitStack() as st:
          if not fast:
            cnd = sb(st, "cnd", [128, 16]); n1 = sb(st, "n1", [128, 16]); n2 = sb(st, "n2", [128, 16])
            modv = sb(st, "modv", [128, 96]); bad = sb(st, "bad", [128, 96])
            wa = [sb(st, "wa%d" % i, [128, 6144]) for i in range(2)]
            pm = [ps(st, "pm%d" % i, [128, 512]) for i in range(2)]
            cdma(cnd, c_in[:, :]); cdma(n1, n1_in[:, :]); cdma(n2, n2_in[:, :]); cdma(bad, bada_in[:, :])
            act(lambda e: e.activation(out=cnd[:], in_=cnd[:], func=AF.Silu), r=[cnd], w=[cnd])
            dve(lambda e: e.tensor_copy(modv[:], bad[:]), r=[bad], w=[modv])
            k = 0
            for kc in range(16):
                for hf in range(2):
                    wb = wa[k % 2]; pb = pm[k % 2]; k += 1
                    dma(wb.t[:], wada_in[kc * 128:(kc + 1) * 128, hf * 6144:(hf + 1) * 6144], w=[wb])
                    for n in range(48):
                        pe(lambda e, n=n, wb=wb, pb=pb, kc=kc: e.matmul(pb[:, n:n + 1], lhsT=wb[:, n * 128:(n + 1) * 128],
                                                                        rhs=cnd[:, kc:kc + 1], start=True, stop=True),
                           r=[wb, cnd], pw=[pb])
                    dve(lambda e, hf=hf, pb=pb: e.tensor_tensor(out=modv[:, hf * 48:(hf + 1) * 48], in0=modv[:, hf * 48:(hf + 1) * 48],
                                                                in1=pb[:, 0:48], op=ALU.add), r=[pb], w=[modv])
            for (scx, bix, gx, nx, o) in ((sc1, bi1, g1, n1, 0), (sc2, bi2, g2, n2, 48)):
                dve(lambda e, scx=scx, nx=nx, o=o: e.scalar_tensor_tensor(out=scx[:], in0=modv[:, o + 16:o + 32], scalar=1.0, in1=nx[:],
                                                                         op0=ALU.add, op1=ALU.mult), r=[modv, nx], w=[scx])
                dve(lambda e, bix=bix, o=o: e.tensor_copy(bix[:], modv[:, o:o + 16]), r=[modv], w=[bix])
                dve(lambda e, gx=gx, o=o: e.tensor_copy(gx[:], modv[:, o + 32:o + 48]), r=[modv], w=[gx])
            if "mod" in dbg:
                dbgt("mod", [128, 96]); dump("mod", modv[:], [modv])
            S.barrier()

        ckpt("mod")
        with contextlib.ExitStack() as st:
          if not fast:
            P = lambda n: sb(st, n, [128, 16])
            are = P("are"); aim = P("aim"); dt_ = P("dt"); th = P("th"); t0 = P("t0"); t1 = P("t1"); t2 = P("t2")
            sn = P("sn"); cs = P("cs"); abr = P("abr"); abi = P("abi"); cfr = P("cfr"); cfi = P("cfi"); den = P("den")
            cdma(are, are_in[:, :]); cdma(aim, aim_in[:, :]); cdma(dt_, ldt_in[:, :])
            act(lambda e: e.activation(out=dt_[:], in_=dt_[:], func=AF.Exp), r=[dt_], w=[dt_])
            dve(lambda e: e.tensor_tensor(out=t0[:], in0=are[:], in1=dt_[:], op=ALU.mult), r=[are, dt_], w=[t0])
            act(lambda e: e.activation(out=mag[:], in_=t0[:], func=AF.Exp), r=[t0], w=[mag])
            dve(lambda e: e.tensor_tensor(out=th[:], in0=aim[:], in1=dt_[:], op=ALU.mult), r=[aim, dt_], w=[th])
            TWO_PI = 2.0 * np.pi
            for kk in (16.0, 8.0, 4.0, 2.0, 1.0):
                dve(lambda e, kk=kk: e.tensor_scalar(t0[:], th[:], float(kk * TWO_PI - np.pi), float(-kk * TWO_PI), ALU.is_gt, ALU.mult),
                    r=[th], w=[t0])
                dve(lambda e: e.tensor_tensor(out=th[:], in0=th[:], in1=t0[:], op=ALU.add), r=[th, t0], w=[th])
            for kk in (1.0,):
                dve(lambda e, kk=kk: e.tensor_scalar(t0[:], th[:], float(-np.pi), float(TWO_PI), ALU.is_lt, ALU.mult), r=[th], w=[t0])
                dve(lambda e: e.tensor_tensor(out=th[:], in0=th[:], in1=t0[:], op=ALU.add), r=[th, t0], w=[th])
            dve(lambda e: e.tensor_scalar(t0[:], th[:], 0.125, None, ALU.mult), r=[th], w=[t0])
            dve(lambda e: e.tensor_tensor(out=t1[:], in0=t0[:], in1=t0[:], op=ALU.mult), r=[t0], w=[t1])
            dve(lambda e: e.tensor_scalar(sn[:], t1[:], -1.0 / 72.0, 1.0, ALU.mult, ALU.add), r=[t1], w=[sn])
            for cst in (42.0, 20.0, 6.0):
                dve(lambda e: e.tensor_tensor(out=sn[:], in0=sn[:], in1=t1[:], op=ALU.mult), r=[sn, t1], w=[sn])
                dve(lambda e, cst=cst: e.tensor_scalar(sn[:], sn[:], -1.0 / cst, 1.0, ALU.mult, ALU.add), r=[sn], w=[sn])
            dve(lambda e: e.tensor_tensor(out=sn[:], in0=sn[:], in1=t0[:], op=ALU.mult), r=[sn, t0], w=[sn])
            dve(lambda e: e.tensor_scalar(cs[:], t1[:], -1.0 / 56.0, 1.0, ALU.mult, ALU.add), r=[t1], w=[cs])
            for cst in (30.0, 12.0, 2.0):
                dve(lambda e: e.tensor_tensor(out=cs[:], in0=cs[:], in1=t1[:], op=ALU.mult), r=[cs, t1], w=[cs])
                dve(lambda e, cst=cst: e.tensor_scalar(cs[:], cs[:], -1.0 / cst, 1.0, ALU.mult, ALU.add), r=[cs], w=[cs])

            def cdouble(cr, ci):
                dve(lambda e: e.tensor_tensor(out=t1[:], in0=cr[:], in1=ci[:], op=ALU.mult), r=[cr, ci], w=[t1])
                dve(lambda e: e.tensor_tensor(out=t2[:], in0=ci[:], in1=ci[:], op=ALU.mult), r=[ci], w=[t2])
                dve(lambda e: e.tensor_tensor(out=cr[:], in0=cr[:], in1=cr[:], op=ALU.mult), r=[cr], w=[cr])
                dve(lambda e: e.tensor_tensor(out=cr[:], in0=cr[:], in1=t2[:], op=ALU.subtract), r=[cr, t2], w=[cr])
                dve(lambda e: e.tensor_scalar(ci[:], t1[:], 2.0, None, ALU.mult), r=[t1], w=[ci])
            for _ in range(3):
                cdouble(cs, sn)
            dve(lambda e: e.tensor_tensor(out=abr[:], in0=mag[:], in1=cs[:], op=ALU.mult), r=[mag, cs], w=[abr])
            dve(lambda e: e.tensor_tensor(out=abi[:], in0=mag[:], in1=sn[:], op=ALU.mult), r=[mag, sn], w=[abi])
            dve(lambda e: e.tensor_tensor(out=den[:], in0=are[:], in1=are[:], op=ALU.mult), r=[are], w=[den])
            dve(lambda e: e.tensor_tensor(out=t0[:], in0=aim[:], in1=aim[:], op=ALU.mult), r=[aim], w=[t0])
            dve(lambda e: e.tensor_tensor(out=den[:], in0=den[:], in1=t0[:], op=ALU.add), r=[den, t0], w=[den])
            dve(lambda e: e.reciprocal(den[:], den[:]), r=[den], w=[den])
            dve(lambda e: e.tensor_scalar(t0[:], abr[:], -1.0, None, ALU.add), r=[abr], w=[t0])
            dve(lambda e: e.tensor_tensor(out=cfr[:], in0=t0[:], in1=are[:], op=ALU.mult), r=[t0, are], w=[cfr])
            dve(lambda e: e.tensor_tensor(out=t1[:], in0=abi[:], in1=aim[:], op=ALU.mult), r=[abi, aim], w=[t1])
            dve(lambda e: e.tensor_tensor(out=cfr[:], in0=cfr[:], in1=t1[:], op=ALU.add), r=[cfr, t1], w=[cfr])
            dve(lambda e: e.tensor_tensor(out=cfr[:], in0=cfr[:], in1=den[:], op=ALU.mult), r=[cfr, den], w=[cfr])
            dve(lambda e: e.tensor_tensor(out=cfi[:], in0=abi[:], in1=are[:], op=ALU.mult), r=[abi, are], w=[cfi])
            dve(lambda e: e.tensor_tensor(out=t1[:], in0=t0[:], in1=aim[:], op=ALU.mult), r=[t0, aim], w=[t1])
            dve(lambda e: e.tensor_tensor(out=cfi[:], in0=cfi[:], in1=t1[:], op=ALU.subtract), r=[cfi, t1], w=[cfi])
            dve(lambda e: e.tensor_tensor(out=cfi[:], in0=cfi[:], in1=den[:], op=ALU.mult), r=[cfi, den], w=[cfi])
            rc = sb(st, "rc", [128, 16, 128]); rsn = sb(st, "rsn", [128, 16, 128])
            pr = P("pr"); pi_ = P("pi")
            dve(lambda e: e.tensor_copy(pr[:], cs[:]), r=[cs], w=[pr])
            dve(lambda e: e.tensor_copy(pi_[:], sn[:]), r=[sn], w=[pi_])
            dve(lambda e: e.memset(rc[:, :, 0:1], 1.0), w=[rc])
            dve(lambda e: e.memset(rsn[:, :, 0:1], 0.0), w=[rsn])
            tmpa = sb(st, "tmpa", [128, 16, 64]); tmpb = sb(st, "tmpb", [128, 16, 64])
            n_ = 1
            while n_ < 128:
                bc = lambda tt_: tt_[:].unsqueeze(2).to_broadcast([128, 16, n_])
                lo = slice(0, n_); hi = slice(n_, 2 * n_)
                dve(lambda e, bc=bc, lo=lo: e.tensor_tensor(out=tmpa[:, :, lo], in0=rc[:, :, lo], in1=bc(pr), op=ALU.mult), r=[rc, pr], w=[tmpa])
                dve(lambda e, bc=bc, lo=lo: e.tensor_tensor(out=tmpb[:, :, lo], in0=rsn[:, :, lo], in1=bc(pi_), op=ALU.mult), r=[rsn, pi_], w=[tmpb])
                dve(lambda e, lo=lo, hi=hi: e.tensor_tensor(out=rc[:, :, hi], in0=tmpa[:, :, lo], in1=tmpb[:, :, lo], op=ALU.subtract), r=[tmpa, tmpb, rsn], w=[rc])
                dve(lambda e, bc=bc, lo=lo: e.tensor_tensor(out=tmpa[:, :, lo], in0=rc[:, :, lo], in1=bc(pi_), op=ALU.mult), r=[rc, pi_], w=[tmpa])
                dve(lambda e, bc=bc, lo=lo: e.tensor_tensor(out=tmpb[:, :, lo], in0=rsn[:, :, lo], in1=bc(pr), op=ALU.mult), r=[rsn, pr], w=[tmpb])
                dve(lambda e, lo=lo, hi=hi: e.tensor_tensor(out=rsn[:, :, hi], in0=tmpa[:, :, lo], in1=tmpb[:, :, lo], op=ALU.add), r=[tmpa, tmpb, rc], w=[rsn])
                cdouble(pr, pi_)
                n_ *= 2
            dve(lambda e: e.tensor_copy(Rre[:], pr[:]), r=[pr], w=[Rre])
            dve(lambda e: e.tensor_copy(Rim[:], pi_[:]), r=[pi_], w=[Rim])
            dma(ssmt.t[:, 0], rc[:], r=[rc], pw=[ssmt]); dma(ssmt.t[:, 1], rsn[:], r=[rsn], pw=[ssmt])
            bre = sb(st, "bre", [128, 16, 128]); bim = sb(st, "bim", [128, 16, 128])
            mre = sb(st, "mre", [128, 16, 128]); mim = sb(st, "mim", [128, 16, 128]); mt = sb(st, "mt", [128, 16, 128])
            cdma(bre, bre_in[:, :, :]); cdma(bim, bim_in[:, :, :])
            bcc = lambda tt_: tt_[:].unsqueeze(2).to_broadcast([128, 16, 128])
            dve(lambda e: e.tensor_tensor(out=mre[:], in0=bre[:], in1=bcc(cfr), op=ALU.mult), r=[bre, cfr], w=[mre])
            dve(lambda e: e.tensor_tensor(out=mt[:], in0=bim[:], in1=bcc(cfi), op=ALU.mult), r=[bim, cfi], w=[mt])
            dve(lambda e: e.tensor_tensor(out=mre[:], in0=mre[:], in1=mt[:], op=ALU.subtract), r=[mre, mt], w=[mre])
            dve(lambda e: e.tensor_tensor(out=mim[:], in0=bim[:], in1=bcc(cfr), op=ALU.mult), r=[bim, cfr], w=[mim])
            dve(lambda e: e.tensor_tensor(out=mt[:], in0=bre[:], in1=bcc(cfi), op=ALU.mult), r=[bre, cfi], w=[mt])
            dve(lambda e: e.tensor_tensor(out=mim[:], in0=mim[:], in1=mt[:], op=ALU.add), r=[mim, mt], w=[mim])
            sw = sb(st, "sw", [128, 4, 16, 128], BF16)
            ptp = [ps(st, "ptp%d" % i, [128, 4, 128]) for i in range(2)]
            k = 0
            for mi, msrc in enumerate((mre, mim)):
                for q4 in range(4):
                    pb = ptp[k % 2]; k += 1
                    for a in range(4):
                        pe(lambda e, a=a, q4=q4, msrc=msrc, pb=pb: e.transpose(out=pb[:, a, :], in_=msrc[:, q4 * 4 + a, :], identity=identf[:]),
                           r=[msrc, identf], pw=[pb])
                    act(lambda e, mi=mi, q4=q4, pb=pb: e.activation(out=sw[:, mi, q4 * 4:(q4 + 1) * 4, :], in_=pb[:], func=AF.Copy), r=[pb], pw=[sw])
            cdma(bre, cre_in[:, :, :]); cdma(bim, cim_in[:, :, :])
            dve(lambda e: e.tensor_copy(sw[:, 2], bre[:]), r=[bre], pw=[sw])
            dve(lambda e: e.tensor_scalar(sw[:, 3], bim[:], -1.0, None, ALU.mult), r=[bim], pw=[sw])
            dma(ssmw.t[:], sw[:], r=[sw], w=[ssmw])
            if "ssmp" in dbg:
                dbgt("ssmp", [128, 6, 16])
                for i_, tt_ in enumerate((abr, abi, cfr, cfi, Rre, Rim)):
                    dump("ssmp", None, None) if False else dma(dbg_out["ssmp"][:, i_, :], tt_[:], r=[tt_])
            S.barrier()

        ckpt("ssmp")
        sw_s = sb(es, "sw_s", [128, 4, 16, 128], BF16)
        dma(sw_s.t[:], ssmw.t[:], r=[ssmw], w=[sw_s])
        if not fast:
            conv_rest()
        ckpt("conv")

        def rmsnorm_to_fm(st, src_tok, scx, bix, hT, nm):
            junk = sb(st, nm + "junk", [128, D], BF16)
            ss = sb(st, nm + "ss", [128, NSUB])
            xn = [sb(st, nm + "xn%d" % i, [128, D]) for i in range(2)]
            ptr = [ps(st, nm + "ptr%d" % i, [128, 4, 128]) for i in range(2)]
            k = 0
            for sub in range(NSUB):
                ap, tt_ = src_tok(sub)
                act(lambda e, ap=ap, sub=sub: e.activation(out=junk[:], in_=ap, func=AF.Square, accum_out=ss[:, sub:sub + 1]),
                    r=[tt_], w=[junk], pw=[ss])
                act(lambda e, sub=sub: e.activation(out=ss[:, sub:sub + 1], in_=ss[:, sub:sub + 1], func=AF.Sqrt, bias=EPS, scale=1.0 / D),
                    r=[ss], pw=[ss])
                dve(lambda e, sub=sub: e.reciprocal(ss[:, sub:sub + 1], ss[:, sub:sub + 1]), r=[ss], pw=[ss])
                ckpt("%s_a%d_%d" % (nm, sub, uid[0] * 0))
                xb = xn[sub % 2]
                pool(lambda e, ap=ap, sub=sub, xb=xb: e.tensor_scalar(xb[:], ap, ss[:, sub:sub + 1], None, ALU.mult), r=[tt_, ss], w=[xb])
                ckpt("%s_b%d_0" % (nm, sub))
                for q4 in range(4):
                    ckpt("%s_e%d_%d" % (nm, sub, q4))
                    pb = ptr[k % 2]; k += 1
                    for a in range(4):
                        dc = q4 * 4 + a
                        pe(lambda e, a=a, dc=dc, xb=xb, pb=pb: e.transpose(out=pb[:, a, :], in_=xb[:, dc * 128:(dc + 1) * 128], identity=identf[:]),
                           r=[xb, identf], pw=[pb])
                    ckpt("%s_t%d_%d" % (nm, sub, q4))
                    for a in range(4):
                        dc = q4 * 4 + a
                        o_ = hT[:, dc, sub * 128:(sub + 1) * 128]
                        if (a % 2 == 0 and EVAC[0] == 2) or EVAC[0] == 1:
                            act(lambda e, o_=o_, a=a, dc=dc, pb=pb: e.activation(out=o_, in_=pb[:, a, :], func=AF.Identity,
                                                                               bias=bix[:, dc:dc + 1], scale=scx[:, dc:dc + 1]),
                                r=[pb, bix, scx], pw=[hT])
                        else:
                            dve(lambda e, o_=o_, a=a, dc=dc, pb=pb: e.tensor_scalar(o_, pb[:, a, :], scx[:, dc:dc + 1], bix[:, dc:dc + 1], ALU.mult, ALU.add),
                                r=[pb, bix, scx], pw=[hT])

        def headnorm_T(st, hT, widx, wbc, dstT, nm, pools):
            wpc, pkv, ptb, kf, ksq, kss, kb_ = pools
            k = 0
            for half in range(2):
                wb = wpc[widx[half] % 2]
                dma(wb.t[:], wp1.t[widx[half]], r=[wp1], w=[wb])
                for sub in range(NSUB):
                    pb = pkv[k % 2]; k += 1
                    for dc in range(16):
                        pe(lambda e, dc=dc, sub=sub, wb=wb, pb=pb: e.matmul(pb[:], lhsT=hT[:, dc, sub * 128:(sub + 1) * 128], rhs=wb[:, dc, :],
                                                                          start=(dc == 0), stop=(dc == 15)), r=[hT, wb], pw=[pb])
                    act(lambda e, pb=pb: e.activation(out=kf[:], in_=pb[:], func=AF.Copy), r=[pb], w=[kf])
                    dve(lambda e: e.tensor_tensor(out=ksq[:], in0=kf[:], in1=kf[:], op=ALU.mult), r=[kf], w=[ksq])
                    dve(lambda e: e.tensor_reduce(out=kss[:], in_=ksq[:].rearrange("p (h d) -> p h d", h=4), axis=AX.X, op=ALU.add), r=[ksq], w=[kss])
                    act(lambda e: e.activation(out=kss[:], in_=kss[:], func=AF.Sqrt, bias=EPS, scale=1.0 / 128), r=[kss], w=[kss])
                    dve(lambda e: e.reciprocal(kss[:], kss[:]), r=[kss], w=[kss])
                    dve(lambda e: e.tensor_tensor(out=kf[:].rearrange("p (h d) -> p h d", h=4), in0=kf[:].rearrange("p (h d) -> p h d", h=4),
                                                  in1=kss[:].unsqueeze(2).to_broadcast([128, 4, 128]), op=ALU.mult), r=[kf, kss], w=[kf])
                    dve(lambda e: e.tensor_tensor(out=kb_[:], in0=kf[:], in1=wbc[:], op=ALU.mult), r=[kf, wbc], w=[kb_])
                    for a in range(4):
                        pe(lambda e, a=a: e.transpose(out=ptb[:, a, :], in_=kb_[:, a * 128:(a + 1) * 128], identity=identb[:]), r=[kb_, identb], pw=[ptb])
                    act(lambda e, half=half, sub=sub: e.activation(out=dstT[:, half * 4:(half + 1) * 4, sub * 128:(sub + 1) * 128], in_=ptb[:], func=AF.Copy),
                        r=[ptb], pw=[dstT])

        for pos in range(npos):
            own = (pos % 4 == 3)
            oi = pos // 4
            tok0 = pos * T
            x1_st = contextlib.ExitStack()
            x1 = sb(x1_st, "x1", [128, NSUB, D]) if own else None
            tile_st = contextlib.ExitStack()
            if True:
                hT = sb(tile_st, "hT", [128, 16, T], BF16)
                uT = sb(tile_st, "uT", [128, 4, T], BF16)
                u32 = sb(tile_st, "u32", [128, 4, T]) if own else None
                yssmT = sb(tile_st, "yssmT", [128, 4, T], BF16) if own else None
                with contextlib.ExitStack() as st:
                    xs = [sb(st, "xs%d" % i, [128, D]) for i in range(2)]
                    if own:
                        for sub in range(NSUB):
                            dma(x1.t[:, sub, :], x_in[tok0 + sub * 128:tok0 + (sub + 1) * 128, :], pw=[x1])
                        src = lambda sub: (x1[:, sub, :], x1)
                    else:
                        def src(sub):
                            xb = xs[sub % 2]
                            dma(xb.t[:], x_in[tok0 + sub * 128:tok0 + (sub + 1) * 128, :], w=[xb])
                            return xb[:], xb
                    rmsnorm_to_fm(st, src, sc1, bi1, hT, "n1")
                    S.barrier()
                if "hT" in dbg and pos == 3:
                    with contextlib.ExitStack() as st:
                        dbgt("hT", [128, 16, T])
                        hdb = sb(st, "hdb", [128, 16, T])
                        dve(lambda e: e.tensor_copy(hdb[:], hT[:]), r=[hT], w=[hdb])
                        dump("hT", hdb[:], [hdb])
                        S.barrier()
                ckpt("norm%d" % pos)
                with contextlib.ExitStack() as st2:
                    wb = sb(st2, "wu", [128, 16, 512], BF16)
                    pkv = [ps(st2, "pkv%d" % i, [128, 512]) for i in range(2)]
                    pf = ps(st2, "pf", [8, T]); pft = ps(st2, "pft", [128, NSUB, 8])
                    dma(wb.t[:], wp1.t[4], r=[wp1], w=[wb])
                    for uc in range(4):
                        pb = pkv[uc % 2]
                        for dc in range(16):
                            pe(lambda e, dc=dc, uc=uc, pb=pb: e.matmul(pb[:], lhsT=wb[:, dc, uc * 128:(uc + 1) * 128], rhs=hT[:, dc, :],
                                                                      start=(dc == 0), stop=(dc == 15)), r=[hT, wb], pw=[pb])
                        if own:
                            act(lambda e, uc=uc, pb=pb: e.activation(out=u32[:, uc, :], in_=pb[:], func=AF.Copy), r=[pb], pw=[u32])
                            dve(lambda e, uc=uc: e.tensor_copy(uT[:, uc, :], u32[:, uc, :]), r=[u32], pw=[uT])
                        else:
                            act(lambda e, uc=uc, pb=pb: e.activation(out=uT[:, uc, :], in_=pb[:], func=AF.Copy), r=[pb], pw=[uT])
                    lf = sb(st2, "lf", [8, T]); fp_ = sb(st2, "fp_", [8, T])
                    dma(fp_.t[:], fpad_in[:, tok0:tok0 + T], w=[fp_])
                    for dc in range(16):
                        pe(lambda e, dc=dc: e.matmul(pf[:], lhsT=wfb[:, dc, :], rhs=hT[:, dc, :], start=(dc == 0), stop=(dc == 15)), r=[hT, wfb], pw=[pf])
                    act(lambda e: e.activation(out=lf[:], in_=pf[:], func=AF.Exp, bias=negbf[:, 0:1], scale=-1.0), r=[pf, negbf], w=[lf])
                    act(lambda e: e.activation(out=lf[:], in_=lf[:], func=AF.Ln, bias=1.0), r=[lf], w=[lf])
                    dve(lambda e: e.tensor_tensor(out=lf[:], in0=lf[:], in1=fp_[:], op=ALU.add), r=[lf, fp_], w=[lf])
                    dve(lambda e: e.tensor_tensor_scan(out=negFT[:], data0=onesT[0:8, :], data1=lf[:], initial=fcar[:, 0:1], op0=ALU.mult, op1=ALU.add),
                        r=[onesT, lf, fcar], w=[negFT])
                    dve(lambda e: e.tensor_copy(fcar[:], negFT[:, T - 1:T]), r=[negFT], w=[fcar])
                    for sub in range(NSUB):
                        pe(lambda e, sub=sub: e.transpose(out=pft[:, sub, :], in_=negFT[:, sub * 128:(sub + 1) * 128], identity=identf[0:8, 0:8]),
                           r=[negFT, identf], pw=[pft])
                    dve(lambda e: e.tensor_copy(negFk[:, pos * NSUB:(pos + 1) * NSUB, :], pft[:]), r=[pft], pw=[negFk])
                    if "negF" in dbg and pos == 3:
                        dbgt("negF", [8, T]); dump("negF", negFT[:], [negFT])
                    S.barrier()
                if True:
                    ckpt("uf%d" % pos)
                    with contextlib.ExitStack() as st2:
                        sw = sw_s; rt = sb(st2, "rt_l", [128, 2, 16, 128])
                        dma(rt.t[:], ssmt.t[:], r=[ssmt], w=[rt])
                        bu = [sb(st2, "bu%d" % i, [128, 2, T]) for i in range(2)]
                        zz = [sb(st2, "zz%d" % i, [128, 2, T]) for i in range(2)]
                        ta = sb(st2, "ta", [128, T]); tb = sb(st2, "tb", [128, T])
                        ww = [sb(st2, "ww%d" % i, [128, 2, T]) for i in range(2)]
                        ini = sb(st2, "ini", [128, 2]); tin = sb(st2, "tin", [128, 2])
                        magT = sb(st2, "magT", [128, 128])
                        sst = sb(st2, "sst", [128, 4, 2, T], BF16) if own else None
                        y32 = sb(st2, "y32", [128, T]) if own else None
                        g32 = sb(st2, "g32", [128, 4, T]) if own else None
                        gb = sb(st2, "gb", [128, 4, T], BF16) if own else None
                        pbu = [ps(st2, "pbu%d" % i, [128, 2, T]) for i in range(2)]
                        py = ps(st2, "py", [128, T]) if own else None
                        r4 = lambda ap: ap.rearrange("p (s l) -> p s l", s=NSUB)
                        for pc in range(16):
                            uc = pc // 4
                            pb = pbu[pc % 2]; bb = bu[pc % 2]; zb = zz[pc % 2]; wv = ww[pc % 2]
                            for ri in range(2):
                                pe(lambda e, ri=ri, pc=pc, uc=uc, pb=pb: e.matmul(pb[:, ri, :], lhsT=sw[:, ri, pc, :], rhs=uT[:, uc, :], start=True, stop=True),
                                   r=[sw, uT], pw=[pb])
                            act(lambda e, pb=pb, bb=bb: e.activation(out=bb[:], in_=pb[:], func=AF.Copy), r=[pb], w=[bb])
                            rcb = rt[:, 0, pc, :].unsqueeze(1).to_broadcast([128, NSUB, 128])
                            rsb = rt[:, 1, pc, :].unsqueeze(1).to_broadcast([128, NSUB, 128])
                            pool(lambda e, bb=bb, rcb=rcb: e.tensor_tensor(out=r4(ta[:]), in0=r4(bb[:, 0, :]), in1=rcb, op=ALU.mult), r=[bb, rt], w=[ta])
                            pool(lambda e, bb=bb, rsb=rsb: e.tensor_tensor(out=r4(tb[:]), in0=r4(bb[:, 1, :]), in1=rsb, op=ALU.mult), r=[bb, rt], w=[tb])
                            pool(lambda e, zb=zb: e.tensor_tensor(out=zb[:, 0, :], in0=ta[:], in1=tb[:], op=ALU.add), r=[ta, tb], pw=[zb])
                            pool(lambda e, bb=bb, rcb=rcb: e.tensor_tensor(out=r4(ta[:]), in0=r4(bb[:, 1, :]), in1=rcb, op=ALU.mult), r=[bb, rt, zb], w=[ta])
                            pool(lambda e, bb=bb, rsb=rsb: e.tensor_tensor(out=r4(tb[:]), in0=r4(bb[:, 0, :]), in1=rsb, op=ALU.mult), r=[bb, rt, zb], w=[tb])
                            pool(lambda e, zb=zb: e.tensor_tensor(out=zb[:, 1, :], in0=ta[:], in1=tb[:], op=ALU.subtract), r=[ta, tb], pw=[zb])
                            dve(lambda e, pc=pc: e.tensor_copy(magT[:], mag[:, pc:pc + 1].to_broadcast([128, 128])), r=[mag], w=[magT])
                            for sub in range(NSUB):
                                gsub = pos * NSUB + sub
                                prev = wend[:, pc, :] if sub == 0 else None
                                pr_re = wend[:, pc, 0:1] if sub == 0 else wv[:, 0, sub * 128 - 1:sub * 128]
                                pr_im = wend[:, pc, 1:2] if sub == 0 else wv[:, 1, sub * 128 - 1:sub * 128]
                                rd = [wend] if sub == 0 else [wv]
                                dve(lambda e, pr_im=pr_im, pc=pc: e.tensor_tensor(out=tin[:, 0:1], in0=pr_im, in1=Rim[:, pc:pc + 1], op=ALU.mult), r=rd + [Rim], w=[tin])
                                dve(lambda e, pr_re=pr_re, pc=pc: e.scalar_tensor_tensor(out=ini[:, 0:1], in0=pr_re, scalar=Rre[:, pc:pc + 1], in1=tin[:, 0:1],
                                                                                         op0=ALU.mult, op1=ALU.subtract), r=rd + [Rre, tin], w=[ini])
                                dve(lambda e, pr_im=pr_im, pc=pc: e.tensor_tensor(out=tin[:, 1:2], in0=pr_im, in1=Rre[:, pc:pc + 1], op=ALU.mult), r=rd + [Rre, ini], w=[tin])
                                dve(lambda e, pr_re=pr_re, pc=pc: e.scalar_tensor_tensor(out=ini[:, 1:2], in0=pr_re, scalar=Rim[:, pc:pc + 1], in1=tin[:, 1:2],
                                                                                         op0=ALU.mult, op1=ALU.add), r=rd + [Rim, tin], pw=[ini])
                                dve(lambda e, gsub=gsub: e.tensor_scalar(ini[:], ini[:], keep[:, gsub:gsub + 1], None, ALU.mult), r=[ini, keep], w=[ini])
                                for ri in range(2):
                                    dve(lambda e, ri=ri, sub=sub, zb=zb, wv=wv: e.tensor_tensor_scan(out=wv[:, ri, sub * 128:(sub + 1) * 128], data0=magT[:],
                                                                                                     data1=zb[:, ri, sub * 128:(sub + 1) * 128], initial=ini[:, ri:ri + 1],
                                                                                                     op0=ALU.mult, op1=ALU.add),
                                        r=[magT, zb, ini], w=[wv])
                            dve(lambda e, pc=pc, wv=wv: e.tensor_copy(wend[:, pc, :], wv[:, :, T - 1]), r=[wv], pw=[wend])
                            if own:
                                a4 = pc % 4
                                pool(lambda e, wv=wv, rcb=rcb: e.tensor_tensor(out=r4(ta[:]), in0=r4(wv[:, 0, :]), in1=rcb, op=ALU.mult), r=[wv, rt], w=[ta])
                                pool(lambda e, wv=wv, rsb=rsb: e.tensor_tensor(out=r4(tb[:]), in0=r4(wv[:, 1, :]), in1=rsb, op=ALU.mult), r=[wv, rt], w=[tb])
                                pool(lambda e, a4=a4: e.tensor_tensor(out=sst[:, a4, 0, :], in0=ta[:], in1=tb[:], op=ALU.subtract), r=[ta, tb], pw=[sst])
                                pool(lambda e, wv=wv, rsb=rsb: e.tensor_tensor(out=r4(ta[:]), in0=r4(wv[:, 0, :]), in1=rsb, op=ALU.mult), r=[wv, rt, sst], w=[ta])
                                pool(lambda e, wv=wv, rcb=rcb: e.tensor_tensor(out=r4(tb[:]), in0=r4(wv[:, 1, :]), in1=rcb, op=ALU.mult), r=[wv, rt, sst], w=[tb])
                                pool(lambda e, a4=a4: e.tensor_tensor(out=sst[:, a4, 1, :], in0=ta[:], in1=tb[:], op=ALU.add), r=[ta, tb], pw=[sst])
                                if a4 == 3:
                                    k = 0
                                    for b4 in range(4):
                                        for ri in range(2):
                                            pe(lambda e, b4=b4, ri=ri, uc=uc, k=k: e.matmul(py[:], lhsT=sw[:, 2 + ri, uc * 4 + b4, :], rhs=sst[:, b4, ri, :],
                                                                                           start=(k == 0), stop=(k == 7)), r=[sw, sst], pw=[py])
                                            k += 1
                                    dve(lambda e, uc=uc: e.scalar_tensor_tensor(out=y32[:], in0=u32[:, uc, :], scalar=dsk[:, uc:uc + 1], in1=py[:],
                                                                                op0=ALU.mult, op1=ALU.add), r=[u32, dsk, py], w=[y32])
                                    act(lambda e, uc=uc: e.activation(out=g32[:, uc, :], in_=y32[:], func=AF.Gelu), r=[y32], pw=[g32])
                                    dve(lambda e, uc=uc: e.tensor_copy(gb[:, uc, :], g32[:, uc, :]), r=[g32], pw=[gb])
                        if own:
                            if "yssm0" in dbg and pos == 3:
                                dbgt("yssm0", [128, 4, T]); dump("yssm0", g32[:], [g32])
                            wg = sb(st2, "wg", [128, 4, 4, 128], BF16)
                            sg = sb(st2, "sg", [128, T])
                            for n in range(4):
                                dma(wg.t[:, n], wgl.t[n], r=[wgl], pw=[wg])
                            for n in range(4):
                                for kc in range(4):
                                    pe(lambda e, n=n, kc=kc: e.matmul(py[:], lhsT=wg[:, n, kc, :], rhs=gb[:, kc, :], start=(kc == 0), stop=(kc == 3)),
                                       r=[wg, gb], pw=[py])
                                act(lambda e, n=n: e.activation(out=sg[:], in_=py[:], func=AF.Sigmoid, bias=bglu[:, n:n + 1]), r=[py, bglu], w=[sg])
                                dve(lambda e, n=n: e.tensor_tensor(out=yssmT[:, n, :], in0=g32[:, n, :], in1=sg[:], op=ALU.mult), r=[g32, sg], pw=[yssmT])
                        S.barrier()
                ckpt("ssm%d" % pos)
                QT = sb(tile_st, "QT", [128, H, T], BF16) if own else None
                with contextlib.ExitStack() as st2:
                    wpc = [sb(st2, "wpc%d" % i, [128, 16, 512], BF16) for i in range(2)]
                    kf = sb(st2, "kf", [128, 512]); ksq = sb(st2, "ksq", [128, 512]); kss = sb(st2, "kss", [128, 4])
                    kb_ = sb(st2, "kb_", [128, 512], BF16)
                    KT = sb(st2, "KT", [128, H, T], BF16); Vb = sb(st2, "Vb", [128, NSUB, 1024], BF16)
                    pkv = [ps(st2, "pkv%d" % i, [128, 512]) for i in range(2)]
                    ptb = ps(st2, "ptb", [128, 4, 128], BF16)
                    headnorm_T(st2, hT, (0, 1), kwb, KT, "k", (wpc, pkv, ptb, kf, ksq, kss, kb_))
                    dma(kts.t[:, :, tok0:tok0 + T], KT[:], r=[KT], pw=[kts])
                    if own:
                        headnorm_T(st2, hT, (5, 6), qwb, QT, "q", (wpc, pkv, ptb, kf, ksq, kss, kb_))
                    k = 0
                    for half in range(2):
                        wb = wpc[half % 2]
                        dma(wb.t[:], wp1.t[2 + half], r=[wp1], w=[wb])
                        for sub in range(NSUB):
                            pb = pkv[k % 2]; k += 1
                            for dc in range(16):
                                pe(lambda e, dc=dc, sub=sub, wb=wb, pb=pb: e.matmul(pb[:], lhsT=hT[:, dc, sub * 128:(sub + 1) * 128], rhs=wb[:, dc, :],
                                                                                  start=(dc == 0), stop=(dc == 15)), r=[hT, wb], pw=[pb])
                            act(lambda e, half=half, sub=sub, pb=pb: e.activation(out=Vb[:, sub, half * 512:(half + 1) * 512], in_=pb[:], func=AF.Copy),
                                r=[pb], pw=[Vb])
                    for sub in range(NSUB):
                        dma(vts.t[:, :, pos * NSUB + sub, :], Vb[:, sub, :].rearrange("p (h d) -> p h d", h=H), r=[Vb], pw=[vts])
                    S.barrier()
                ckpt("kv%d" % pos)
                if not own:
                    tile_st.close(); x1_st.close()
                    continue

                yattnT = sb(tile_st, "yattnT", [128, H, T], BF16)
                with contextlib.ExitStack() as st:
                    Kc = [sb(st, "Kc%d" % i, [128, 1024], BF16) for i in range(2)]
                    Vc = [sb(st, "Vc%d" % i, [128, 8, 129], BF16) for i in range(2)]
                    fqb = [sb(st, "fqb%d" % i, [128, T]) for i in range(2)]
                    sS = [sb(st, "sS%d" % i, [128, T]) for i in range(2)]
                    PT = [sb(st, "PT%d" % i, [128, T], BF16) for i in range(2)]
                    yat = sb(st, "yat", [128, NSUB, 1024], BF16)
                    rinv = sb(st, "rinv", [128, 1])
                    pst = [ps(st, "pst%d" % i, [128, T]) for i in range(2)]
                    po = [ps(st, "po%d" % i, [128, 512]) for i in range(NSUB)]
                    pfq = ps(st, "pfq", [128, T])
                    ptr = ps(st, "patr", [128, 4, 128], BF16)
                    for vb in Vc:
                        dve(lambda e, vb=vb: e.memset(vb[:, :, 128:129], 1.0), pw=[vb])
                    nkb = (pos + 1) * NSUB
                    blk = 0; chunk = 0
                    for h in range(H):
                        fq = fqb[h % 2]
                        dve(lambda e, h=h: e.tensor_copy(selh[:], negI[:, h:h + 1].to_broadcast([8, 128])), r=[negI], w=[selh])
                        pe(lambda e, h=h: e.matmul(pfq[:], lhsT=selh[:], rhs=negFT[:], start=True, stop=True), r=[selh, negFT], w=[pfq])
                        act(lambda e, fq=fq: e.activation(out=fq[:], in_=pfq[:], func=AF.Copy), r=[pfq], w=[fq])
                        for kb in range(nkb):
                            if kb % 8 == 0:
                                kc_ = Kc[chunk % 2]; vc_ = Vc[chunk % 2]; chunk += 1
                                n8 = min(8, nkb - kb)
                                dma(kc_.t[:, 0:n8 * 128], kts.t[:, h, kb * 128:(kb + n8) * 128], r=[kts], w=[kc_])
                                dma(vc_.t[:, 0:n8, 0:128], vts.t[:, h, kb:kb + n8, :], r=[vts], pw=[vc_])
                            qlo = max(0, kb - pos * NSUB)
                            c0 = qlo * 128
                            pS = pst[blk % 2]; s_ = sS[blk % 2]; p_ = PT[blk % 2]; blk += 1
                            pe(lambda e, h=h, kb=kb, c0=c0, kc_=kc_, pS=pS: e.matmul(pS[:, c0:T], lhsT=kc_[:, (kb % 8) * 128:(kb % 8 + 1) * 128], rhs=QT[:, h, c0:T],
                                                                                     start=True, stop=True), r=[kc_, QT], w=[pS])
                            dve(lambda e, h=h, kb=kb, c0=c0, pS=pS, s_=s_, fq=fq: e.scalar_tensor_tensor(out=s_[:, c0:T], in0=pS[:, c0:T], scalar=negFk[:, kb, h:h + 1],
                                                                                                       in1=fq[:, c0:T], op0=ALU.add, op1=ALU.add),
                                r=[pS, negFk, fq], w=[s_])
                            if kb >= pos * NSUB:
                                dve(lambda e, c0=c0, s_=s_: e.tensor_tensor(out=s_[:, c0:c0 + 128], in0=s_[:, c0:c0 + 128], in1=maskT[:], op=ALU.add), r=[s_, maskT], w=[s_])
                            act(lambda e, c0=c0, s_=s_, p_=p_: e.activation(out=p_[:, c0:T], in_=s_[:, c0:T], func=AF.Exp), r=[s_], w=[p_])
                            for sub in range(qlo, NSUB):
                                last = pos * NSUB + sub
                                pe(lambda e, sub=sub, kb=kb, p_=p_, vc_=vc_, last=last: e.matmul(po[sub][:, 0:129], lhsT=p_[:, sub * 128:(sub + 1) * 128], rhs=vc_[:, kb % 8, :],
                                                                                              start=(kb == 0), stop=(kb == last)), r=[p_, vc_], pw=[po[sub]])
                        for sub in range(NSUB):
                            dve(lambda e, sub=sub: e.reciprocal(rinv[:], po[sub][:, 128:129]), r=[po[sub]], w=[rinv])
                            dve(lambda e, sub=sub, h=h: e.tensor_scalar(yat[:, sub, h * 128:(h + 1) * 128], po[sub][:, 0:128], rinv[:, 0:1], None, ALU.mult),
                                r=[po[sub], rinv], pw=[yat])
                    for sub in range(NSUB):
                        for half in range(2):
                            for a in range(4):
                                h = half * 4 + a
                                pe(lambda e, a=a, h=h, sub=sub: e.transpose(out=ptr[:, a, :], in_=yat[:, sub, h * 128:(h + 1) * 128], identity=identb[:]),
                                   r=[yat, identb], pw=[ptr])
                            act(lambda e, half=half, sub=sub: e.activation(out=yattnT[:, half * 4:(half + 1) * 4, sub * 128:(sub + 1) * 128], in_=ptr[:], func=AF.Copy),
                                r=[ptr], pw=[yattnT])
                    if "yattn" in dbg and pos == 3:
                        dbgt("yattn", [128, H, T])
                        ydb = sb(st, "ydb", [128, H, T])
                        dve(lambda e: e.tensor_copy(ydb[:], yattnT[:]), r=[yattnT], w=[ydb])
                        dump("yattn", ydb[:], [ydb])
                    S.barrier()

                ckpt("attn%d" % pos)
                def add_back(st, srcT, gx, nm):
                    dT = [sb(st, nm + "dT%d" % i, [128, T]) for i in range(2)]
                    pt_ = [ps(st, nm + "pt%d" % i, [128, NSUB, 128]) for i in range(2)]
                    for n in range(16):
                        d_ = dT[n % 2]; pb = pt_[n % 2]
                        sap, stt = srcT(n)
                        act(lambda e, n=n, d_=d_, sap=sap: e.activation(out=d_[:], in_=sap, func=AF.Copy, scale=gx[:, n:n + 1]), r=[stt, gx], w=[d_])
                        for sub in range(NSUB):
                            pe(lambda e, sub=sub, d_=d_, pb=pb: e.transpose(out=pb[:, sub, :], in_=d_[:, sub * 128:(sub + 1) * 128], identity=identf[:]),
                               r=[d_, identf], pw=[pb])
                        dve(lambda e, n=n, pb=pb: e.tensor_tensor(out=x1[:, :, n * 128:(n + 1) * 128], in0=x1[:, :, n * 128:(n + 1) * 128], in1=pb[:], op=ALU.add),
                            r=[pb], pw=[x1])

                with contextlib.ExitStack() as st:
                    merged = sb(st, "merged", [128, 16, T], BF16)
                    wmb = [sb(st, "wmb%d" % i, [128, 44, 128], BF16) for i in range(2)]
                    sgs = sb(st, "sgs", [128, T]); sga = sb(st, "sga", [128, T]); m1 = sb(st, "m1", [128, T]); m2 = sb(st, "m2", [128, T])
                    pmg = [ps(st, "pmg%d" % i, [128, T]) for i in range(4)]
                    for n in range(16):
                        wb = wmb[n % 2]
                        dma(wb.t[:], wm.t[n], r=[wm], w=[wb])
                        for kc in range(4):
                            pe(lambda e, kc=kc, wb=wb: e.matmul(pmg[0][:], lhsT=wb[:, kc, :], rhs=yssmT[:, kc, :], start=(kc == 0), stop=(kc == 3)), r=[wb, yssmT], pw=[pmg[0]])
                        for kc in range(8):
                            pe(lambda e, kc=kc, wb=wb: e.matmul(pmg[1][:], lhsT=wb[:, 4 + kc, :], rhs=yattnT[:, kc, :], start=(kc == 0), stop=(kc == 7)), r=[wb, yattnT], pw=[pmg[1]])
                        for kc in range(16):
                            pe(lambda e, kc=kc, wb=wb: e.matmul(pmg[2][:], lhsT=wb[:, 12 + kc, :], rhs=hT[:, kc, :], start=(kc == 0), stop=(kc == 15)), r=[wb, hT], pw=[pmg[2]])
                        for kc in range(16):
                            pe(lambda e, kc=kc, wb=wb: e.matmul(pmg[3][:], lhsT=wb[:, 28 + kc, :], rhs=hT[:, kc, :], start=(kc == 0), stop=(kc == 15)), r=[wb, hT], pw=[pmg[3]])
                        act(lambda e: e.activation(out=sgs[:], in_=pmg[2][:], func=AF.Sigmoid), r=[pmg[2]], w=[sgs])
                        act(lambda e: e.activation(out=sga[:], in_=pmg[3][:], func=AF.Sigmoid), r=[pmg[3]], w=[sga])
                        dve(lambda e: e.tensor_tensor(out=m1[:], in0=sgs[:], in1=pmg[0][:], op=ALU.mult), r=[sgs, pmg[0]], w=[m1])
                        dve(lambda e: e.tensor_tensor(out=m2[:], in0=sga[:], in1=pmg[1][:], op=ALU.mult), r=[sga, pmg[1]], w=[m2])
                        pool(lambda e, n=n: e.tensor_tensor(out=merged[:, n, :], in0=m1[:], in1=m2[:], op=ALU.add), r=[m1, m2], pw=[merged])
                    S.barrier()
                    with contextlib.ExitStack() as st2:
                        wob = [sb(st2, "wob%d" % i, [128, 16, 128], BF16) for i in range(2)]
                        pop = [ps(st2, "pop%d" % i, [128, T]) for i in range(2)]

                        def srcT(n):
                            wb = wob[n % 2]; pb = pop[n % 2]
                            dma(wb.t[:], wo.t[n], r=[wo], w=[wb])
                            for kc in range(16):
                                pe(lambda e, kc=kc, wb=wb, pb=pb: e.matmul(pb[:], lhsT=wb[:, kc, :], rhs=merged[:, kc, :], start=(kc == 0), stop=(kc == 15)),
                                   r=[wb, merged], pw=[pb])
                            return pb[:], pb
                        add_back(st2, srcT, g1, "o")
                        S.barrier()
                    S.barrier()
                if "x1" in dbg and pos == 3:
                    dbgt("x1", [128, NSUB, D]); dump("x1", x1[:], [x1])

                ckpt("merge%d" % pos)
                S.barrier()
                tile_st.close()
                with contextlib.ExitStack() as st:
                    h2T = sb(st, "h2T", [128, 16, T], BF16)
                    with contextlib.ExitStack() as st2:
                        rmsnorm_to_fm(st2, lambda sub: (x1[:, sub, :], x1), sc2, bi2, h2T, "n2")
                        S.barrier()
                    scr_ = sb(st, "scr_", [128, NSUB, 16, 128])
                    thr = sb(st, "thr", [128, NSUB, H]); negc = sb(st, "negc", [128, NSUB, H])
                    with contextlib.ExitStack() as st2:
                        qT = sb(st2, "qT", [128, 16, T], BF16)
                        wqb = [sb(st2, "wqb%d" % i, [128, 16, 128], BF16) for i in range(2)]
                        pq = [ps(st2, "pq%d" % i, [128, T]) for i in range(2)]
                        psc = [ps(st2, "psc%d" % i, [128, 4, 128]) for i in range(2)]
                        for c in range(16):
                            wb = wqb[c % 2]; pb = pq[c % 2]
                            dma(wb.t[:], wqs.t[c], r=[wqs], w=[wb])
                            for kc in range(16):
                                pe(lambda e, kc=kc, wb=wb, pb=pb: e.matmul(pb[:], lhsT=wb[:, kc, :], rhs=h2T[:, kc, :], start=(kc == 0), stop=(kc == 15)), r=[wb, h2T], pw=[pb])
                            act(lambda e, c=c, pb=pb: e.activation(out=qT[:, c, :], in_=pb[:], func=AF.Copy), r=[pb], pw=[qT])
                        k = 0
                        for sub in range(NSUB):
                            for q4 in range(4):
                                pb = psc[k % 2]; k += 1
                                for a in range(4):
                                    c = q4 * 4 + a
                                    pe(lambda e, a=a, c=c, sub=sub, pb=pb: e.matmul(pb[:, a, :], lhsT=qT[:, c, sub * 128:(sub + 1) * 128], rhs=keysb[:, c, :], start=True, stop=True),
                                       r=[qT, keysb], pw=[pb])
                                act(lambda e, sub=sub, q4=q4, pb=pb: e.activation(out=scr_[:, sub, q4 * 4:(q4 + 1) * 4, :], in_=pb[:], func=AF.Copy), r=[pb], pw=[scr_])
                        v16 = sb(st2, "v16", [128, 2, 16]); tmp128 = sb(st2, "tmp128", [128, 128])
                        cand = sb(st2, "cand", [128, 256]); cand2 = sb(st2, "cand2", [128, 256]); tv = sb(st2, "tv", [128, 16])
                        ez = sb(st2, "ez", [128, 16]); zs = sb(st2, "zs", [128, 1]); nm_ = sb(st2, "nm_", [128, 1])
                        for sub in range(NSUB):
                            for h in range(H):
                                for p in range(2):
                                    sv = scr_[:, sub, h * 2 + p, :]
                                    dve(lambda e, p=p, sv=sv: e.max(out=v16[:, p, 0:8], in_=sv), r=[scr_], pw=[v16])
                                    dve(lambda e, p=p, sv=sv: e.match_replace(out=tmp128[:], in_to_replace=v16[:, p, 0:8], in_values=sv, imm_value=-1e30), r=[scr_, v16], w=[tmp128])
                                    dve(lambda e, p=p: e.max(out=v16[:, p, 8:16], in_=tmp128[:]), r=[tmp128], pw=[v16])
                                dve(lambda e: e.tensor_tensor(out=cand[:].rearrange("p (a b) -> p a b", a=16), in0=v16[:, 0, :].unsqueeze(2).to_broadcast([128, 16, 16]),
                                                              in1=v16[:, 1, :].unsqueeze(1).to_broadcast([128, 16, 16]), op=ALU.add), r=[v16], w=[cand])
                                dve(lambda e: e.max(out=tv[:, 0:8], in_=cand[:]), r=[cand], pw=[tv])
                                dve(lambda e: e.match_replace(out=cand2[:], in_to_replace=tv[:, 0:8], in_values=cand[:], imm_value=-1e30), r=[cand, tv], w=[cand2])
                                dve(lambda e: e.max(out=tv[:, 8:16], in_=cand2[:]), r=[cand2], pw=[tv])
                                dve(lambda e, sub=sub, h=h: e.tensor_copy(thr[:, sub, h:h + 1], tv[:, 15:16]), r=[tv], pw=[thr])
                                dve(lambda e: e.tensor_scalar(nm_[:], tv[:, 0:1], -1.0, None, ALU.mult), r=[tv], w=[nm_])
                                act(lambda e: e.activation(out=ez[:], in_=tv[:], func=AF.Exp, bias=nm_[:, 0:1], accum_out=zs[:, 0:1]), r=[tv, nm_], w=[ez, zs])
                                act(lambda e: e.activation(out=zs[:], in_=zs[:], func=AF.Ln), r=[zs], w=[zs])
                                dve(lambda e, sub=sub, h=h: e.tensor_tensor(out=negc[:, sub, h:h + 1], in0=nm_[:], in1=zs[:], op=ALU.subtract), r=[nm_, zs], pw=[negc])
                        S.barrier()
                    acc = sb(st, "acc", [128, 16, T])
                    WTs = [sb(st, "WT%d" % i, [128, GE, T], BF16) for i in range(2)]
                    Sgs = [sb(st, "Sg%d" % i, [128, GE, 128]) for i in range(2)]
                    Egs = [sb(st, "Eg%d" % i, [128, GE, 128], BF16) for i in range(2)]
                    Gts = [sb(st, "Gt%d" % i, [128, GE, 128], BF16) for i in range(2)]
                    Gas = [sb(st, "Ga%d" % i, [128, GE, 128], BF16) for i in range(2)]
                    kgat = 0
                    ub = [sb(st, "ub%d" % i, [128, 16, 128], BF16) for i in range(3)]
                    vb_ = [sb(st, "vb%d" % i, [128, GE, 128], BF16) for i in range(2)]
                    pa = [ps(st, "pa%d" % i, [128, T]) for i in range(2)]
                    pg = [ps(st, "pg%d" % i, [128, 4, 128], BF16) for i in range(2)]
                    pv = [ps(st, "pv%d" % i, [128, T]) for i in range(2)]
                    pusv = pus.t.rearrange("(i p) (c e) -> i p c e", p=128, c=16)
                    pvsv = pvs.t.rearrange("(g c p) (i d) -> g c p i d", c=16, p=128, i=GE)
                    ku = 0; kv = 0; kg = 0
                    for g in range(NG):
                        WT = WTs[g % 2]
                        for il in range(GE):
                            i = g * GE + il
                            u_ = ub[ku % 3]; pb = pa[ku % 2]; ku += 1
                            dma(u_.t[:], pusv[i], r=[pus], w=[u_])
                            for dc in range(16):
                                pe(lambda e, dc=dc, u_=u_, pb=pb: e.matmul(pb[:], lhsT=u_[:, dc, :], rhs=h2T[:, dc, :], start=(dc == 0), stop=(dc == 15)), r=[u_, h2T], pw=[pb])
                            act(lambda e, il=il, pb=pb: e.activation(out=WT[:, il, :], in_=pb[:], func=AF.Gelu), r=[pb], pw=[WT])
                        for sub in range(NSUB):
                            Ga = Gas[(g * NSUB + sub) % 2]
                            for h in range(H):
                                Sg = Sgs[kgat % 2]; Eg = Egs[kgat % 2]; Gt = Gts[kgat % 2]; kgat += 1
                                s1 = scr_[:, sub, h * 2, g * GE:(g + 1) * GE].unsqueeze(2).to_broadcast([128, GE, 128])
                                s2 = scr_[:, sub, h * 2 + 1, :].unsqueeze(1).to_broadcast([128, GE, 128])
                                dve(lambda e, s1=s1, s2=s2: e.tensor_tensor(out=Sg[:], in0=s1, in1=s2, op=ALU.add), r=[scr_], w=[Sg])
                                act(lambda e, sub=sub, h=h: e.activation(out=Eg[:], in_=Sg[:], func=AF.Exp, bias=negc[:, sub, h:h + 1]), r=[Sg, negc], w=[Eg])
                                gdst = Ga if h == 0 else Gt
                                dve(lambda e, sub=sub, h=h, gdst=gdst: e.scalar_tensor_tensor(out=gdst[:].rearrange("p a b -> p (a b)"), in0=Sg[:].rearrange("p a b -> p (a b)"),
                                                                                              scalar=thr[:, sub, h:h + 1], in1=Eg[:].rearrange("p a b -> p (a b)"),
                                                                                              op0=ALU.is_ge, op1=ALU.mult), r=[Sg, Eg, thr], w=[gdst])
                                if h > 0:
                                    pool(lambda e: e.tensor_tensor(out=Ga[:], in0=Ga[:], in1=Gt[:], op=ALU.add), r=[Ga, Gt], w=[Ga])
                            for q4 in range(GE // 4):
                                pb = pg[kg % 2]; kg += 1
                                for a in range(4):
                                    pe(lambda e, a=a, q4=q4, pb=pb: e.transpose(out=pb[:, a, :], in_=Ga[:, q4 * 4 + a, :], identity=identb[:]), r=[Ga, identb], pw=[pb])
                                dve(lambda e, q4=q4, sub=sub, pb=pb: e.tensor_tensor(out=WT[:, q4 * 4:(q4 + 1) * 4, sub * 128:(sub + 1) * 128],
                                                                                    in0=WT[:, q4 * 4:(q4 + 1) * 4, sub * 128:(sub + 1) * 128], in1=pb[:], op=ALU.mult),
                                    r=[pb, WT], pw=[WT])
                        for dc in range(16):
                            v_ = vb_[kv % 2]; pb = pv[kv % 2]; kv += 1
                            dma(v_.t[:], pvsv[g, dc], r=[pvs], w=[v_])
                            for il in range(GE):
                                pe(lambda e, il=il, v_=v_, pb=pb: e.matmul(pb[:], lhsT=v_[:, il, :], rhs=WT[:, il, :], start=(il == 0), stop=(il == GE - 1)), r=[v_, WT], pw=[pb])
                            if g == 0:
                                act(lambda e, dc=dc, pb=pb: e.activation(out=acc[:, dc, :], in_=pb[:], func=AF.Copy), r=[pb], pw=[acc])
                            else:
                                dve(lambda e, dc=dc, pb=pb: e.tensor_tensor(out=acc[:, dc, :], in0=acc[:, dc, :], in1=pb[:], op=ALU.add), r=[pb, acc], pw=[acc])
                    S.barrier()
                    with contextlib.ExitStack() as st2:
                        add_back(st2, lambda n: (acc[:, n, :], acc), g2, "p")
                        S.barrier()
                    for sub in range(NSUB):
                        dma(out_t[oi * T + sub * 128:oi * T + (sub + 1) * 128, :], x1[:, sub, :], r=[x1])
                    S.barrier()
                S.barrier()
                x1_st.close()
        S.barrier()
        print("bass instructions:", S.ninst, "sems:", S.nsem)
    return nc, list(dbg_out)


def _prep_inputs(inp):
    f = np.float32
    g = lambda k: np.asarray(inp[k], dtype=f)
    pm = lambda v, n: np.ascontiguousarray(v.reshape(n, 128).T)
    common = {}
    common["w_ada"] = np.ascontiguousarray(g("w_ada")[0])
    common["b_ada"] = pm(g("b_ada")[0], 96)
    common["norm1_w"] = pm(g("norm1_w")[0], 16)
    common["norm2_w"] = pm(g("norm2_w")[0], 16)
    common["w_in"] = np.ascontiguousarray(g("w_in")[0])
    common["negbf"] = np.ascontiguousarray(-g("b_forget")[0].reshape(8, 1)) if False else None
    common["qw_bc"] = np.ascontiguousarray(np.tile(g("q_norm_w")[0][None, :], (128, 4)))
    common["kw_bc"] = np.ascontiguousarray(np.tile(g("k_norm_w")[0][None, :], (128, 4)))
    common["a_re"] = pm(g("ssm_A_re")[0].reshape(-1), 16)
    common["a_im"] = pm(g("ssm_A_im")[0].reshape(-1), 16)
    common["log_dt"] = pm(np.repeat(g("ssm_log_dt")[0], 64), 16)

    def placeB(B):
        o = np.zeros((128, 16, 128), f)
        for gi in range(32):
            o[(gi % 2) * 64:(gi % 2) * 64 + 64, gi // 2, (gi % 8) * 16:(gi % 8) * 16 + 16] = B[gi]
        return o
    common["b_re_pl"] = placeB(g("ssm_B_re")[0]); common["b_im_pl"] = placeB(g("ssm_B_im")[0])
    common["c_re_pl"] = placeB(g("ssm_C_re")[0].transpose(0, 2, 1)); common["c_im_pl"] = placeB(g("ssm_C_im")[0].transpose(0, 2, 1))
    common["d_skip"] = pm(g("ssm_D")[0].reshape(-1), 4)
    common["w_glu"] = np.ascontiguousarray(g("w_glu")[0]); common["b_glu"] = pm(g("b_glu")[0], 4)
    common["w_ssm_up"] = np.ascontiguousarray(g("w_ssm_up")[0]); common["w_attn_up"] = np.ascontiguousarray(g("w_attn_up")[0])
    common["w_out"] = np.ascontiguousarray(g("w_out")[0]); common["w_peer_q"] = np.ascontiguousarray(g("w_peer_q")[0])
    common["keysT"] = np.ascontiguousarray(g("peer_sub_keys")[0].reshape(16, 128, 128).transpose(2, 0, 1))
    pu = g("peer_u")[0].reshape(128, 128, 16, 128)
    common["peer_uT"] = np.ascontiguousarray(pu.transpose(0, 3, 2, 1)).reshape(128 * 128, 2048)
    pv = g("peer_v")[0].reshape(NG, GE, 128, 16, 128)
    common["peer_vL"] = np.ascontiguousarray(pv.transpose(0, 3, 2, 1, 4)).reshape(NG * 16 * 128, GE * 128)
    common["ident"] = np.eye(128, dtype=f)
    kk = np.arange(128)
    common["maskT"] = np.where(kk[:, None] <= kk[None, :], 0.0, -30000.0).astype(f)
    del common["negbf"]
    common["negbf"] = np.ascontiguousarray(g("b_forget")[0].reshape(8, 1))
    maps = []
    x = g("x"); c = g("c")
    for core in range(8):
        b, j = core // 4, core % 4
        pad = (3 - j) * T
        m = dict(common)
        xs = np.zeros((NPOS * T, D), f)
        xs[pad:] = x[b, :NPOS * T - pad]
        m["x"] = xs
        m["c"] = pm(c[b], 16)
        fp = np.zeros((8, NPOS * T), f)
        kp = np.ones((128, NPOS * NSUB), f)
        if pad > 0:
            fp[:, pad] = PADF
            kp[:, pad // 128] = 0.0
        m["fpad"] = fp; m["keep"] = kp
        maps.append(m)
    return maps


def kernel(**inputs):
    maps = _prep_inputs(inputs)
    nc, _ = build()
    res = run_bass_kernel_spmd(nc, maps, core_ids=list(range(8)))
    out = np.zeros((2, SEQ, D), np.float32)
    for core in range(8):
        b, j = core // 4, core % 4
        o = np.asarray(res.results[core]["out"], dtype=np.float32).reshape(NPOS // 4, T, D)
        for m in range(NPOS // 4):
            t = 4 * m + j
            out[b, t * T:(t + 1) * T] = o[m]
    return out
```
